# Optimizing a Trainium2 kernel written in Bass

```python
import jax, jax.numpy as jnp
from jax import lax
import numpy as np

D_MODEL = 1024
BATCH = 8
SEQ = 4096
DEPTH = 4

NSA_HEADS = 8
NSA_KV_GROUPS = 2
NSA_HEAD_DIM = 64
NSA_BRANCHES = 3
CMP_BLOCK = 32
CMP_STRIDE = 16
CMP_HIDDEN = 256
SEL_BLOCK = 64
N_SELECT = 16
N_LOCAL = 2
WINDOW = 512
NSA_Q_CHUNK = 64

MLA_HEADS = 8
MLA_NOPE_DIM = 64
MLA_ROPE_DIM = 32
MLA_V_DIM = 64
Q_LORA_RANK = 256
KV_LORA_RANK = 128
ATTN_BLOCK = 128

D_FF = 4 * D_MODEL
ROPE_THETA = 10000.0
NORM_EPS = 1e-6
NEG_INF = -1e30
FORCE_BONUS = 1e4

NSA_WIDTH = NSA_HEADS * NSA_HEAD_DIM
NSA_KV_WIDTH = NSA_KV_GROUPS * NSA_HEAD_DIM
MLA_WIDTH = MLA_HEADS * MLA_V_DIM
MIX_WIDTH = NSA_WIDTH + MLA_WIDTH
IN_SIZES = (NSA_WIDTH, NSA_KV_WIDTH, NSA_KV_WIDTH, NSA_KV_WIDTH, NSA_KV_WIDTH,
            NSA_KV_WIDTH, NSA_KV_WIDTH, NSA_BRANCHES * NSA_HEADS,
            Q_LORA_RANK, KV_LORA_RANK, MLA_ROPE_DIM)
IN_WIDTH = sum(IN_SIZES)

kernel_name = "hymba_nsa_mla_hybrid_trunk"


def _rms_norm(x, gain):
    xf = x.astype(jnp.float32)
    y = xf * lax.rsqrt(jnp.mean(xf * xf, axis=-1, keepdims=True) + NORM_EPS)
    return (y * gain.astype(jnp.float32)).astype(x.dtype)


def _rope_tables(seq, dim, dtype):
    inv_freq = 1.0 / (ROPE_THETA ** (jnp.arange(0, dim, 2, dtype=jnp.float32) / dim))
    ang = jnp.arange(seq, dtype=jnp.float32)[:, None] * inv_freq[None, :]
    ang = jnp.concatenate([ang, ang], axis=-1)
    return jnp.cos(ang).astype(dtype), jnp.sin(ang).astype(dtype)


def _rope(x, cos, sin):
    half = x.shape[-1] // 2
    rot = jnp.concatenate([-x[..., half:], x[..., :half]], axis=-1)
    return x * cos[:, None, :] + rot * sin[:, None, :]


def _masked_softmax(s, mask):
    s = jnp.where(mask, s, NEG_INF)
    m = jnp.max(s, axis=-1, keepdims=True)
    e = jnp.where(mask, jnp.exp(s - m), 0.0)
    return e / jnp.maximum(jnp.sum(e, axis=-1, keepdims=True), 1e-30)


def _heads(t, n):
    b, s, w = t.shape
    return t.reshape(b, s, n, w // n)


def _compress(kv, pos_emb, w1, w2):
    B, S, G, dk = kv.shape
    ratio = CMP_BLOCK // CMP_STRIDE
    n_cmp = S // CMP_STRIDE - ratio + 1
    chunks = kv.reshape(B, S // CMP_STRIDE, CMP_STRIDE, G, dk)
    blocks = jnp.concatenate([chunks[:, i:i + n_cmp] for i in range(ratio)], axis=2)
    blocks = blocks + pos_emb[None, None, :, None, :]
    flat = jnp.swapaxes(blocks, 2, 3).reshape(B, n_cmp, G, CMP_BLOCK * dk)
    return jax.nn.silu(flat @ w1) @ w2


def _nsa(q, k_cmp, v_cmp, k_sel, v_sel, k_win, v_win, gates):
    B, S, H, dk = q.shape
    G = k_sel.shape[2]
    R = H // G
    n_cmp = k_cmp.shape[1]
    n_blk = S // SEL_BLOCK
    n_sel = min(N_SELECT, n_blk)
    QC = NSA_Q_CHUNK
    scale = dk ** -0.5
    qg = q.reshape(B, S, G, R, dk)
    gg = gates.reshape(B, S, G, R, NSA_BRANCHES)
    k_blocks = k_sel.reshape(B, n_blk, SEL_BLOCK, G, dk).transpose(0, 3, 1, 2, 4)
    v_blocks = v_sel.reshape(B, n_blk, SEL_BLOCK, G, dk).transpose(0, 3, 1, 2, 4)
    pad = ((0, 0), (WINDOW, 0), (0, 0), (0, 0))
    k_win_pad = jnp.pad(k_win, pad)
    v_win_pad = jnp.pad(v_win, pad)
    cmp_end = jnp.arange(n_cmp) * CMP_STRIDE + CMP_BLOCK - 1
    blk_ids = jnp.arange(n_blk)
    a = SEL_BLOCK // CMP_STRIDE
    b = CMP_BLOCK // CMP_STRIDE
    overlap = np.convolve(np.ones(a), np.ones(b))
    bi = jnp.arange(B)[:, None, None, None]
    gi = jnp.arange(G)[None, :, None, None]

    def chunk(c):
        s0 = c * QC
        t = s0 + jnp.arange(QC)
        qc = lax.dynamic_slice_in_dim(qg, s0, QC, axis=1)
        gc = lax.dynamic_slice_in_dim(gg, s0, QC, axis=1)
        s_cmp = jnp.einsum('bqgrd,bngd->bgrqn', qc, k_cmp, preferred_element_type=jnp.float32) * scale
        p_cmp = _masked_softmax(s_cmp, cmp_end[None, :] <= t[:, None])
        o_cmp = jnp.einsum('bgrqn,bngd->bqgrd', p_cmp.astype(v_cmp.dtype), v_cmp)
        imp = jnp.pad(p_cmp.sum(axis=2), ((0, 0), (0, 0), (0, 0), (b - 1, b - 1)))
        imp_blk = sum(float(overlap[j]) * imp[..., j:j + a * (n_blk - 1) + 1:a] for j in range(a + b - 1))
        cur = t // SEL_BLOCK
        valid = blk_ids[None, :] <= cur[:, None]
        forced = valid & ((blk_ids[None, :] == 0) | (blk_ids[None, :] > cur[:, None] - N_LOCAL))
        score = jnp.where(forced, FORCE_BONUS, jnp.where(valid, imp_blk, -1.0))
        _, idx = lax.top_k(score, n_sel)
        k_g = k_blocks[bi, gi, idx]
        v_g = v_blocks[bi, gi, idx]
        key_pos = idx[..., None] * SEL_BLOCK + jnp.arange(SEL_BLOCK)
        m_sel = (key_pos <= t[:, None, None]).reshape(B, G, 1, QC, n_sel * SEL_BLOCK)
        s_sel = jnp.einsum('bqgrd,bgqnkd->bgrqnk', qc, k_g, preferred_element_type=jnp.float32) * scale
        p_sel = _masked_softmax(s_sel.reshape(B, G, R, QC, n_sel * SEL_BLOCK), m_sel)
        o_sel = jnp.einsum('bgrqj,bgqjd->bqgrd', p_sel.astype(v_g.dtype),
                           v_g.reshape(B, G, QC, n_sel * SEL_BLOCK, dk))
        kw = lax.dynamic_slice_in_dim(k_win_pad, s0, WINDOW + QC, axis=1)
        vw = lax.dynamic_slice_in_dim(v_win_pad, s0, WINDOW + QC, axis=1)
        win_pos = s0 - WINDOW + jnp.arange(WINDOW + QC)
        m_win = ((win_pos[None, :] <= t[:, None]) & (win_pos[None, :] > t[:, None] - WINDOW)
                 & (win_pos[None, :] >= 0))
        s_win = jnp.einsum('bqgrd,bkgd->bgrqk', qc, kw, preferred_element_type=jnp.float32) * scale
        p_win = _masked_softmax(s_win, m_win)
        o_win = jnp.einsum('bgrqk,bkgd->bqgrd', p_win.astype(vw.dtype), vw)
        return gc[..., 0:1] * o_cmp + gc[..., 1:2] * o_sel + gc[..., 2:3] * o_win

    out = lax.map(chunk, jnp.arange(S // QC))
    return jnp.swapaxes(out, 0, 1).reshape(B, S, H * dk)


def _causal_attention(q, k, v):
    B, S, H, dk = q.shape
    dv = v.shape[-1]
    scale = dk ** -0.5
    kpos = jnp.arange(S)

    def block(c):
        s0 = c * ATTN_BLOCK
        qb = lax.dynamic_slice_in_dim(q, s0, ATTN_BLOCK, axis=1)
        s = jnp.einsum('bqhd,bkhd->bhqk', qb, k, preferred_element_type=jnp.float32) * scale
        mask = kpos[None, :] <= (s0 + jnp.arange(ATTN_BLOCK))[:, None]
        p = _masked_softmax(s, mask)
        return jnp.einsum('bhqk,bkhd->bqhd', p.astype(v.dtype), v)

    out = lax.map(block, jnp.arange(S // ATTN_BLOCK))
    return jnp.swapaxes(out, 0, 1).reshape(B, S, H, dv)


def _mla(c_q, c_kv, k_rope, q_norm, w_q_up, kv_norm, w_kv_up, cos, sin):
    B, S, _ = c_q.shape
    q = (_rms_norm(c_q, q_norm) @ w_q_up).reshape(B, S, MLA_HEADS, MLA_NOPE_DIM + MLA_ROPE_DIM)
    q = jnp.concatenate([q[..., :MLA_NOPE_DIM], _rope(q[..., MLA_NOPE_DIM:], cos, sin)], axis=-1)
    kv = (_rms_norm(c_kv, kv_norm) @ w_kv_up).reshape(B, S, MLA_HEADS, MLA_NOPE_DIM + MLA_V_DIM)
    k_nope, v = kv[..., :MLA_NOPE_DIM], kv[..., MLA_NOPE_DIM:]
    k_pe = _rope(k_rope[:, :, None, :], cos, sin)
    k = jnp.concatenate([k_nope, jnp.broadcast_to(k_pe, (B, S, MLA_HEADS, MLA_ROPE_DIM))], axis=-1)
    return _causal_attention(q, k, v).reshape(B, S, MLA_WIDTH)


def setup_inputs(seed: int = 0) -> dict:
    key = jax.random.key(seed)
    ks = jax.random.split(key, 20)
    L = DEPTH
    dk = NSA_HEAD_DIM

    def dense(k, shape, fan_in):
        return jax.random.normal(k, shape, jnp.float32) * fan_in ** -0.5

    def gain(k, shape):
        return 1.0 + 0.02 * jax.random.normal(k, shape, jnp.float32)

    return {
        "x": jax.random.normal(ks[0], (BATCH, SEQ, D_MODEL), jnp.float32),
        "attn_norm": gain(ks[1], (L, D_MODEL)),
        "w_in": dense(ks[2], (L, D_MODEL, IN_WIDTH), D_MODEL),
        "cmp_pos_k": 0.1 * jax.random.normal(ks[3], (L, CMP_BLOCK, dk), jnp.float32),
        "cmp_w1_k": dense(ks[4], (L, CMP_BLOCK * dk, CMP_HIDDEN), CMP_BLOCK * dk),
        "cmp_w2_k": dense(ks[5], (L, CMP_HIDDEN, dk), CMP_HIDDEN),
        "cmp_pos_v": 0.1 * jax.random.normal(ks[6], (L, CMP_BLOCK, dk), jnp.float32),
        "cmp_w1_v": dense(ks[7], (L, CMP_BLOCK * dk, CMP_HIDDEN), CMP_BLOCK * dk),
        "cmp_w2_v": dense(ks[8], (L, CMP_HIDDEN, dk), CMP_HIDDEN),
        "mla_q_norm": gain(ks[9], (L, Q_LORA_RANK)),
        "w_q_up": dense(ks[10], (L, Q_LORA_RANK, MLA_HEADS * (MLA_NOPE_DIM + MLA_ROPE_DIM)), Q_LORA_RANK),
        "mla_kv_norm": gain(ks[11], (L, KV_LORA_RANK)),
        "w_kv_up": dense(ks[12], (L, KV_LORA_RANK, MLA_HEADS * (MLA_NOPE_DIM + MLA_V_DIM)), KV_LORA_RANK),
        "nsa_out_norm": gain(ks[13], (L, NSA_WIDTH)),
        "mla_out_norm": gain(ks[14], (L, MLA_WIDTH)),
        "w_out": dense(ks[15], (L, MIX_WIDTH, D_MODEL), MIX_WIDTH),
        "mlp_norm": gain(ks[16], (L, D_MODEL)),
        "w_ff1": dense(ks[17], (L, D_MODEL, D_FF), D_MODEL),
        "w_ff2": dense(ks[18], (L, D_FF, D_MODEL), D_FF),
        "final_norm": gain(ks[19], (D_MODEL,)),
    }


def reference(x, attn_norm, w_in, cmp_pos_k, cmp_w1_k, cmp_w2_k, cmp_pos_v, cmp_w1_v, cmp_w2_v,
              mla_q_norm, w_q_up, mla_kv_norm, w_kv_up, nsa_out_norm, mla_out_norm, w_out,
              mlp_norm, w_ff1, w_ff2, final_norm):
    B, S, _ = x.shape
    cos_a, sin_a = _rope_tables(S, NSA_HEAD_DIM, x.dtype)
    cos_b, sin_b = _rope_tables(S, MLA_ROPE_DIM, x.dtype)
    offsets = [int(o) for o in np.cumsum(IN_SIZES)[:-1]]
    G = NSA_KV_GROUPS
    for l in range(DEPTH):
        h = _rms_norm(x, attn_norm[l])
        proj = h @ w_in[l]
        (q_a, k_c, v_c, k_s, v_s, k_w, v_w, g_a, c_q, c_kv, k_r) = jnp.split(proj, offsets, axis=-1)
        q_a = _rope(_heads(q_a, NSA_HEADS), cos_a, sin_a)
        k_cmp = _compress(_rope(_heads(k_c, G), cos_a, sin_a), cmp_pos_k[l], cmp_w1_k[l], cmp_w2_k[l])
        v_cmp = _compress(_heads(v_c, G), cmp_pos_v[l], cmp_w1_v[l], cmp_w2_v[l])
        gates = jax.nn.sigmoid(g_a).reshape(B, S, NSA_HEADS, NSA_BRANCHES)
        o_a = _nsa(q_a, k_cmp, v_cmp,
                   _rope(_heads(k_s, G), cos_a, sin_a), _heads(v_s, G),
                   _rope(_heads(k_w, G), cos_a, sin_a), _heads(v_w, G), gates)
        o_b = _mla(c_q, c_kv, k_r, mla_q_norm[l], w_q_up[l], mla_kv_norm[l], w_kv_up[l], cos_b, sin_b)
        mixed = jnp.concatenate([_rms_norm(o_a, nsa_out_norm[l]), _rms_norm(o_b, mla_out_norm[l])], axis=-1)
        x = x + mixed @ w_out[l]
        h = _rms_norm(x, mlp_norm[l])
        x = x + jnp.square(jax.nn.relu(h @ w_ff1[l])) @ w_ff2[l]
    return _rms_norm(x, final_norm)
```

```python
import numpy as np
from contextlib import ExitStack
import concourse.bass as bass
import concourse.mybir as mybir
from concourse.bass_utils import run_bass_kernel_spmd

F32 = mybir.dt.float32
BF16 = mybir.dt.bfloat16
ALU = mybir.AluOpType
AF = mybir.ActivationFunctionType
AX = mybir.AxisListType

D = 1024
NH = 8
G = 2
DK = 64
HID = 4096
RQ = 256
RKV = 128
NEG = -30000.0
EPS = 1e-6
WINC = 19 * 128 + 280
TM0 = 19 * 128


class Buf:
    __slots__ = ("name", "w", "r", "psum")

    def __init__(self, name="", psum=False):
        self.name = name
        self.w = None
        self.r = []
        self.psum = psum


class Op:
    __slots__ = ("eng", "fn", "deps", "dma", "flag", "sem", "val")

    def __init__(self, eng, fn, dma):
        self.eng = eng
        self.fn = fn
        self.dma = dma
        self.deps = []
        self.flag = False
        self.sem = None
        self.val = 0


class Prog:
    ENGS = ("pe", "act", "dve", "pool", "sp")
    NDS = 12

    def __init__(self, nc, es):
        self.nc = nc
        self.ops = {e: [] for e in self.ENGS}
        self.esem = {e: es.enter_context(nc.semaphore("s_" + e)) for e in ("pe", "act", "dve", "pool")}
        self.dsem = {e: [es.enter_context(nc.semaphore("d_%s%d" % (e, i))) for i in range(self.NDS)]
                     for e in ("sp", "pool")}
        self.ecnt = {e: 0 for e in self.ENGS}
        self.dk = {e: 0 for e in self.ENGS}
        self.dcount = {e: [0] * self.NDS for e in ("sp", "pool")}
        self.known = {e: {} for e in self.ENGS}
        self.ninst = 0
        self.ecnt_done = {}

    def op(self, eng, fn, reads=(), writes=(), dma=False):
        o = Op(eng, fn, dma)
        deps = {}
        for b in reads:
            if b.w is not None:
                deps[id(b.w)] = (b.w, True)
            if b.psum:
                for r in b.r:
                    if r.eng != eng and id(r) not in deps:
                        deps[id(r)] = (r, False)
        for b in writes:
            if b.w is not None and id(b.w) not in deps:
                deps[id(b.w)] = (b.w, False)
            for r in b.r:
                if id(r) not in deps:
                    deps[id(r)] = (r, False)
        for d, raw in deps.values():
            if d is o:
                continue
            if d.dma or dma:
                o.deps.append(d)
            elif d.eng == eng:
                if eng != "pe":
                    o.deps.append(d)
            else:
                o.deps.append(d)
        for b in reads:
            b.r.append(o)
        for b in writes:
            b.w = o
            b.r = []
        self.ops[eng].append(o)
        return o

    def dma(self, eng, out, in_, reads=(), writes=()):
        return self.op(eng, lambda e: e.dma_start(out=out, in_=in_), reads, writes, dma=True)

    def flush(self):
        nc = self.nc
        lasts = []
        for e in self.ENGS:
            for o in reversed(self.ops[e]):
                if not o.dma and o.fn is not None:
                    lasts.append(o)
                    break
        alld = [o for e in self.ENGS for o in self.ops[e] if o.dma]
        for e in self.ENGS:
            b = Op(e, None, False)
            b.deps = list(lasts) + alld
            self.ops[e].append(b)
        for e in self.ENGS:
            for o in self.ops[e]:
                for d in o.deps:
                    d.flag = True
        for e in self.ENGS:
            for o in self.ops[e]:
                if o.dma:
                    s = self.dk[e] % self.NDS
                    self.dk[e] += 1
                    self.dcount[e][s] += 16
                    o.sem = self.dsem[e][s]
                    o.val = self.dcount[e][s]
                    o.flag = True
                elif o.flag and o.fn is not None:
                    self.ecnt[e] += 1
                    o.sem = self.esem[e]
                    o.val = self.ecnt[e]
        with nc.Block() as block:
            engobj = {"pe": block.tensor, "act": block.scalar, "dve": block.vector,
                      "pool": block.gpsimd, "sp": block.sync}

            def make_body(e):
                ops = self.ops[e]
                known = self.known[e]

                def body(eng):
                    emitted = [self.ecnt_done.get(e, 0)]
                    for o in ops:
                        need = {}
                        for d in o.deps:
                            if d.sem is None:
                                continue
                            key = id(d.sem)
                            if known.get(key, 0) >= d.val:
                                continue
                            if key not in need or need[key][1] < d.val:
                                need[key] = (d.sem, d.val)
                        if o.dma:
                            key = id(o.sem)
                            pv = o.val - 16
                            if pv > 0 and known.get(key, 0) < pv:
                                if key not in need or need[key][1] < pv:
                                    need[key] = (o.sem, pv)
                        for key, (s, v) in need.items():
                            if e in self.esem and s is self.esem[e]:
                                v = max(v, emitted[0] - 2)
                            eng.wait_ge(s, v)
                            known[key] = v
                            self.ninst += 1
                        if o.fn is None:
                            continue
                        ins = o.fn(eng)
                        self.ninst += 1
                        if o.flag:
                            ins.then_inc(o.sem, 16 if o.dma else 1)
                            if not o.dma:
                                emitted[0] = o.val
                return body

            for e in self.ENGS:
                if self.ops[e]:
                    engobj[e](make_body(e))
        self.ecnt_done = dict(self.ecnt)
        self.ops = {e: [] for e in self.ENGS}


class Ring:
    def __init__(self, tiles):
        self.tiles = tiles
        self.bufs = [Buf() for _ in tiles]
        self.i = 0

    def next(self):
        k = self.i % len(self.tiles)
        self.i += 1
        return self.tiles[k], self.bufs[k]


def _rope_tables(S):
    t = np.arange(S, dtype=np.float32)[:, None]
    invA = (1.0 / (np.float32(10000.0) ** (np.arange(0, 64, 2, dtype=np.float32) / np.float32(64)))).astype(np.float32)
    angA = (t * invA[None, :]).astype(np.float32)
    cosA = np.cos(angA).astype(np.float32)
    sinA = np.sin(angA).astype(np.float32)
    ropeA = np.zeros((2, 128, S), np.float32)
    for r0 in (0, 64):
        ropeA[0, r0:r0 + 32] = cosA.T
        ropeA[0, r0 + 32:r0 + 64] = cosA.T
        ropeA[1, r0:r0 + 32] = -sinA.T
        ropeA[1, r0 + 32:r0 + 64] = sinA.T
    invB = (1.0 / (np.float32(10000.0) ** (np.arange(0, 32, 2, dtype=np.float32) / np.float32(32)))).astype(np.float32)
    angB = (t * invB[None, :]).astype(np.float32)
    cosB = np.cos(angB).astype(np.float32)
    sinB = np.sin(angB).astype(np.float32)
    ropeB = np.zeros((128, S), np.float32)
    ropeB[0:16] = -sinB.T
    ropeB[16:32] = sinB.T
    ropeB[32:48] = cosB.T
    ropeB[48:64] = cosB.T
    ropeB[64:80] = cosB.T
    ropeB[80:96] = cosB.T
    return ropeA, ropeB


def _consts(S):
    c = {}
    c["identB"] = np.eye(128, dtype=np.float32)
    k = np.arange(128)[:, None]
    q = np.arange(128)[None, :]
    q5 = np.arange(512)[None, :]
    c["triC"] = np.concatenate([np.where(128 * j + k <= q5, 0.0, NEG) for j in range(4)], axis=1).astype(np.float32)
    c["triW"] = np.concatenate([np.where(q5 < 128 * j + k, 0.0, NEG) for j in range(4)], axis=1).astype(np.float32)
    c["eind"] = (np.arange(S)[None, :] // 64 == np.arange(64)[:, None]).astype(np.float32)
    i = np.arange(33)[:, None]
    ql = np.arange(512)[None, :]
    c["cmT"] = np.where(16 * (i - 2) + 31 <= ql, 0.0, NEG).astype(np.float32)
    c["jbig"] = (np.arange(400)[None, :] == i + 256).astype(np.float32)
    p = np.arange(128)[:, None]
    cc = np.arange(128)[None, :]
    d = cc - 62
    hi = (p >= 64).astype(np.int64)
    valid = d <= hi
    cur = d == hi
    prev = d == hi - 1
    forced = cur | prev
    c["selv"] = (valid & ~forced).astype(np.float32)
    c["sela"] = np.where(cur, 10000.0, np.where(prev, 10001.0, np.where(valid, 0.0, -1.0))).astype(np.float32)
    return c


def _win_cols():
    idx = []
    rot = lambda b: list(range(b + 32, b + 64)) + list(range(b, b + 32))
    for p in range(4):
        b0, b1 = 64 * (2 * p), 64 * (2 * p + 1)
        idx += list(range(b0, b0 + 64)) + list(range(b1, b1 + 64))
        idx += rot(b0) + rot(b1)
    for kb in (512, 768, 1024):
        idx += list(range(kb, kb + 128))
        idx += rot(kb) + rot(kb + 64)
    idx += list(range(640, 768))
    idx += list(range(1304, 1560))
    idx += list(range(1560, 1688))
    b = 1688
    idx += list(range(b + 16, b + 32)) + list(range(b, b + 16)) + list(range(b, b + 32)) + [1720] * 64
    idx += list(range(896, 1024)) + list(range(1152, 1280)) + list(range(1280, 1304))
    assert len(idx) == WINC
    return np.array(idx)


def _wq_cols():
    idx = []
    for h in range(8):
        b = 96 * h
        idx += list(range(b, b + 96)) + list(range(b + 80, b + 96)) + list(range(b + 64, b + 80))
    return np.array(idx)


def _wkv_cols():
    idx = []
    for h in range(8):
        idx += list(range(128 * h, 128 * h + 64))
    for h in range(8):
        idx += list(range(128 * h + 64, 128 * h + 128))
    return np.array(idx)


def _pk(g):
    L, n = g.shape
    return np.ascontiguousarray(g.reshape(L, n // 128, 128).transpose(0, 2, 1))


def build(S, DEPTH):
    NT = S // 128
    NG = S // 512
    NCMP = S // 16 - 1
    NBLK = S // 64
    nc = bass.Bass("TRN2", target_bir_lowering=False)

    def din(name, shape, dt=F32):
        return nc.dram_tensor(name, list(shape), dt, kind="ExternalInput").ap()

    def dsc(name, shape, dt=BF16):
        return nc.dram_tensor(name, list(shape), dt).ap()

    x_in = din("x", [S, D])
    w_in = din("w_in", [DEPTH, D, WINC])
    g_attn = din("g_attn", [DEPTH, 128, 8])
    w1k = din("w1k", [DEPTH, 2048, 256])
    w1v = din("w1v", [DEPTH, 2048, 256])
    w2k = din("w2k", [DEPTH, 256, 64])
    w2v = din("w2v", [DEPTH, 256, 64])
    posk = din("posk", [DEPTH, 64, 32])
    posv = din("posv", [DEPTH, 64, 32])
    wq = din("wq", [DEPTH, RQ, 1024])
    g_q = din("g_q", [DEPTH, 128, 2])
    wkv = din("wkv", [DEPTH, RKV, 1024])
    g_kv = din("g_kv", [DEPTH, 128, 1])
    w_out = din("w_out", [DEPTH, D, D])
    g_mix = din("g_mix", [DEPTH, 128, 8])
    w_ff1 = din("w_ff1", [DEPTH, D, HID])
    g_mlp = din("g_mlp", [DEPTH, 128, 8])
    w_ff2 = din("w_ff2", [DEPTH, HID, D])
    g_fin = din("g_fin", [128, D])
    ropeA = din("ropeA", [2, 128, S])
    ropeB = din("ropeB", [128, S])
    cshape = {"identB": [128, 128], "triC": [128, 2048], "triW": [128, 2048], "eind": [64, S],
              "cmT": [33, 512], "jbig": [33, 400]}
    cin = {k: din("c_" + k, v) for k, v in cshape.items()}
    selv = din("c_selv", [128, 128])
    sela = din("c_sela", [128, 128])
    out = nc.dram_tensor("out", [S, D], F32, kind="ExternalOutput").ap()

    cb = {k: dsc("b_" + k, v) for k, v in cshape.items()}
    Win_b = dsc("Win_b", [DEPTH, D, WINC])
    W1k_b = dsc("W1k_b", [DEPTH, 2048, 256])
    W1v_b = dsc("W1v_b", [DEPTH, 2048, 256])
    W2k_b = dsc("W2k_b", [DEPTH, 256, 64])
    W2v_b = dsc("W2v_b", [DEPTH, 256, 64])
    Pk_b = dsc("Pk_b", [DEPTH, 64, 32])
    Pv_b = dsc("Pv_b", [DEPTH, 64, 32])
    Wq_b = dsc("Wq_b", [DEPTH, RQ, 1024])
    Wkv_b = dsc("Wkv_b", [DEPTH, RKV, 1024])
    Wout_b = dsc("Wout_b", [DEPTH, D, D])
    W1_b = dsc("W1_b", [DEPTH, D, HID])
    W2_b = dsc("W2_b", [DEPTH, HID, D])
    xs = dsc("xs", [S, D], F32)
    QaT = dsc("QaT", [NH, 64, S])
    KcT = dsc("KcT", [128, S])
    VcT = dsc("VcT", [128, S])
    KsT = dsc("KsT", [G, 64, S])
    KwT = dsc("KwT", [G, 64, S])
    Vs = dsc("Vs", [S, 128])
    Vw = dsc("Vw", [S, 128])
    gates = dsc("gates", [S, 24], F32)
    cqnT = dsc("cqnT", [RQ, S])
    ckvnT = dsc("ckvnT", [RKV, S])
    krT = dsc("krT", [32, S])
    KcmpT = dsc("KcmpT", [G, 64, 256])
    Vcmp = dsc("Vcmp", [256, G, 64])
    mixed = dsc("mixed", [S, D])
    h2T = dsc("h2T", [D, S])
    obD = dsc("obD", [S, 512], F32)

    top = ExitStack()
    P = Prog(nc, top)
    PS = [top.enter_context(nc.psum_tensor("ps%d" % i, [128, 512], F32)) for i in range(8)]
    PB = [Buf("ps%d" % i, psum=True) for i in range(8)]
    uid = [0]

    def sbt(st, shape, dt=F32):
        uid[0] += 1
        return st.enter_context(nc.sbuf_tensor("t%d" % uid[0], list(shape), dt))

    def mm(o, l, r, start, stop, reads, writes):
        return P.op("pe", lambda e: e.matmul(o, lhsT=l, rhs=r, start=start, stop=stop), reads, writes)

    def tr(o, i, idn, reads, writes):
        return P.op("pe", lambda e: e.transpose(out=o, in_=i, identity=idn), reads, writes)

    def act(o, i, func, reads, writes, scale=1.0, bias=0.0, accum=None):
        if accum is None:
            return P.op("act", lambda e: e.activation(out=o, in_=i, func=func, bias=bias, scale=scale), reads, writes)
        return P.op("act", lambda e: e.activation(out=o, in_=i, func=func, bias=bias, scale=scale, accum_out=accum),
                    reads, writes)

    def tt(eng, o, a, b, op, reads, writes):
        return P.op(eng, lambda e: e.tensor_tensor(out=o, in0=a, in1=b, op=op), reads, writes)

    def ts(eng, o, a, s1, s2, op0, op1, reads, writes):
        if s2 is None:
            return P.op(eng, lambda e: e.tensor_scalar(out=o, in0=a, scalar1=s1, scalar2=None, op0=op0), reads, writes)
        return P.op(eng, lambda e: e.tensor_scalar(out=o, in0=a, scalar1=s1, scalar2=s2, op0=op0, op1=op1), reads, writes)

    def stt(eng, o, a, s, b, op0, op1, reads, writes):
        return P.op(eng, lambda e: e.scalar_tensor_tensor(out=o, in0=a, scalar=s, in1=b, op0=op0, op1=op1), reads, writes)

    def cp(eng, o, i, reads, writes):
        if eng == "act":
            return act(o, i, AF.Copy, reads, writes)
        return P.op(eng, lambda e: e.tensor_copy(out=o, in_=i), reads, writes)

    def ms(eng, o, v, writes):
        return P.op(eng, lambda e: e.memset(o, v), (), writes)

    def recip(o, i, reads, writes):
        return P.op("dve", lambda e: e.reciprocal(out=o, in_=i), reads, writes)

    def rstd_of(o, ss, n, reads, writes):
        act(o, ss, AF.Ln, reads, writes, scale=1.0 / n, bias=EPS)
        act(o, o, AF.Exp, writes, writes, scale=-0.5)

    def phase_W():
        with ExitStack() as st:
            CW = 2048
            stg = Ring([sbt(st, [128, CW], F32) for _ in range(3)])
            outb = Ring([sbt(st, [128, CW], BF16) for _ in range(3)])
            gt = sbt(st, [128, DEPTH, 32], F32)
            bg = Buf()
            for l in range(DEPTH):
                P.dma("sp", gt[:, l, 0:8], g_attn[l], writes=[bg])
                P.dma("sp", gt[:, l, 8:10], g_q[l], writes=[bg])
                P.dma("sp", gt[:, l, 10:11], g_kv[l], writes=[bg])
                P.dma("sp", gt[:, l, 11:19], g_mix[l], writes=[bg])
                P.dma("sp", gt[:, l, 19:27], g_mlp[l], writes=[bg])
            engs = ["dve", "act"]
            cnt = [0]

            def conv(src, dst, rows, cols, gain):
                for c0 in range(0, cols, CW):
                    cw = min(CW, cols - c0)
                    s_t, s_b = stg.next()
                    o_t, o_b = outb.next()
                    P.dma("sp", s_t[0:rows, 0:cw], src[:, c0:c0 + cw], writes=[s_b])
                    e = engs[cnt[0] % 2]
                    cnt[0] += 1
                    if gain is None:
                        cp(e, o_t[0:rows, 0:cw], s_t[0:rows, 0:cw], [s_b], [o_b])
                    elif e == "act":
                        act(o_t[0:rows, 0:cw], s_t[0:rows, 0:cw], AF.Copy, [s_b, bg], [o_b], scale=gain)
                    else:
                        ts(e, o_t[0:rows, 0:cw], s_t[0:rows, 0:cw], gain, None, ALU.mult, None, [s_b, bg], [o_b])
                    P.dma("pool", dst[:, c0:c0 + cw], o_t[0:rows, 0:cw], reads=[o_b])

            for k, shp in cshape.items():
                conv(cin[k], cb[k], shp[0], shp[1], None)
            for l in range(DEPTH):
                for k in range(8):
                    conv(w_in[l, k * 128:(k + 1) * 128, :], Win_b[l, k * 128:(k + 1) * 128, :], 128, WINC, gt[:, l, k:k + 1])
                for k in range(16):
                    conv(w1k[l, k * 128:(k + 1) * 128, :], W1k_b[l, k * 128:(k + 1) * 128, :], 128, 256, None)
                    conv(w1v[l, k * 128:(k + 1) * 128, :], W1v_b[l, k * 128:(k + 1) * 128, :], 128, 256, None)
                for k in range(2):
                    conv(w2k[l, k * 128:(k + 1) * 128, :], W2k_b[l, k * 128:(k + 1) * 128, :], 128, 64, None)
                    conv(w2v[l, k * 128:(k + 1) * 128, :], W2v_b[l, k * 128:(k + 1) * 128, :], 128, 64, None)
                    conv(wq[l, k * 128:(k + 1) * 128, :], Wq_b[l, k * 128:(k + 1) * 128, :], 128, 1024, gt[:, l, 8 + k:9 + k])
                conv(posk[l], Pk_b[l], 64, 32, None)
                conv(posv[l], Pv_b[l], 64, 32, None)
                conv(wkv[l], Wkv_b[l], 128, 1024, gt[:, l, 10:11])
                for k in range(8):
                    conv(w_out[l, k * 128:(k + 1) * 128, :], Wout_b[l, k * 128:(k + 1) * 128, :], 128, D, gt[:, l, 11 + k:12 + k])
                    conv(w_ff1[l, k * 128:(k + 1) * 128, :], W1_b[l, k * 128:(k + 1) * 128, :], 128, HID, gt[:, l, 19 + k:20 + k])
                for k in range(32):
                    conv(w_ff2[l, k * 128:(k + 1) * 128, :], W2_b[l, k * 128:(k + 1) * 128, :], 128, D, None)
            P.flush()

    def phase_A(l):
        xsrc = x_in if l == 0 else xs
        with ExitStack() as st:
            Wt = sbt(st, [128, 8, WINC], BF16)
            bW = Buf()
            for k in range(8):
                P.dma("sp", Wt[:, k, :], Win_b[l, k * 128:(k + 1) * 128, :], writes=[bW])
            ident = sbt(st, [128, 128], BF16)
            bI = Buf()
            P.dma("sp", ident[:], cb["identB"], writes=[bI])
            ones = sbt(st, [128, 128], BF16)
            bO = Buf()
            ms("pool", ones[:], 1.0, [bO])
            xr = Ring([sbt(st, [128, D], F32) for _ in range(3)])
            xnr = Ring([sbt(st, [128, D], BF16) for _ in range(5)])
            junk = sbt(st, [128, D], BF16)
            bJ = Buf()
            statr = Ring([sbt(st, [128, 4], F32) for _ in range(5)])
            hTr = Ring([sbt(st, [128, 8, 512], BF16) for _ in range(2)])
            rAr = Ring([sbt(st, [128, 2, 512], F32) for _ in range(2)])
            rBr = Ring([sbt(st, [128, 512], F32) for _ in range(2)])
            t1r = Ring([sbt(st, [128, 512], F32) for _ in range(3)])
            t2r = Ring([sbt(st, [128, 512], F32) for _ in range(3)])
            obr = Ring([sbt(st, [128, 512], BF16) for _ in range(4)])
            cqr = Ring([sbt(st, [128, 3, 512], F32) for _ in range(2)])
            sqr = Ring([sbt(st, [128, 3, 512], BF16) for _ in range(2)])
            rsr = Ring([sbt(st, [128, 512], F32) for _ in range(2)])
            tmr = Ring([sbt(st, [128, 280], BF16) for _ in range(3)])
            gtr = Ring([sbt(st, [128, 24], F32) for _ in range(3)])
            ptr = Ring([PS[0], PS[1]]); ptr.bufs = [PB[0], PB[1]]
            pfr = Ring([PS[2], PS[3], PS[4], PS[5]]); pfr.bufs = [PB[2], PB[3], PB[4], PB[5]]
            pmr = Ring([PS[6], PS[7]]); pmr.bufs = [PB[6], PB[7]]
            hts = {}

            def normT(tg):
                c0 = tg * 512
                hT, bH = hTr.next()
                rA, bRA = rAr.next()
                rB, bRB = rBr.next()
                P.dma("sp", rA[:, 0, :], ropeA[0, :, c0:c0 + 512], writes=[bRA])
                P.dma("sp", rA[:, 1, :], ropeA[1, :, c0:c0 + 512], writes=[bRA])
                P.dma("sp", rB[:], ropeB[:, c0:c0 + 512], writes=[bRB])
                xns = []
                for j in range(4):
                    t = tg * 4 + j
                    xt, bX = xr.next()
                    P.dma("sp", xt[:], xsrc[t * 128:(t + 1) * 128, :], writes=[bX])
                    sx, bS = statr.next()
                    act(junk[:], xt[:], AF.Square, [bX], [bJ, bS], accum=sx[:, 0:1])
                    rstd_of(sx[:, 1:2], sx[:, 0:1], D, [bS], [bS])
                    xn, bN = xnr.next()
                    ts("dve", xn[:], xt[:], sx[:, 1:2], None, ALU.mult, None, [bX, bS], [bN])
                    xns.append((xn, bN))
                hts[tg] = (hT, bH, rA, bRA, rB, bRB, xns)

            def normT2(tg):
                hT, bH, rA, bRA, rB, bRB, xns = hts[tg]
                for j in range(4):
                    xn, bN = xns[j]
                    pt, bP = ptr.next()
                    ptb = pt[:].bitcast(BF16)
                    for k in range(8):
                        tr(ptb[:, k * 128:(k + 1) * 128], xn[:, k * 128:(k + 1) * 128], ident[:], [bN, bI], [bP])
                    cp("act" if j % 2 else "dve", hT[:, :, j * 128:(j + 1) * 128],
                       ptb[:, 0:1024].rearrange("p (k t) -> p k t", k=8), [bP], [bH])

            def proj(tg):
                c0 = tg * 512
                hT, bH, rA, bRA, rB, bRB, _xns = hts.pop(tg)
                cq, bCQ = cqr.next()
                sq, bSQ = sqr.next()
                QaTf = QaT.rearrange("h d s -> (h d) s")
                KsTf = KsT.rearrange("g d s -> (g d) s")
                KwTf = KwT.rearrange("g d s -> (g d) s")
                for ct in range(19):
                    if ct < 14 and ct % 2 == 1:
                        continue
                    pf, bF = pfr.next()
                    for k in range(8):
                        mm(pf[:], Wt[:, k, ct * 128:(ct + 1) * 128], hT[:, k, :], k == 0, k == 7, [bW, bH], [bF])
                    if ct < 14:
                        pg, bG2 = pfr.next()
                        for k in range(8):
                            mm(pg[:], Wt[:, k, (ct + 1) * 128:(ct + 2) * 128], hT[:, k, :], k == 0, k == 7, [bW, bH], [bG2])
                        t1, b1 = t1r.next()
                        t2, b2 = t2r.next()
                        ob, bOB = obr.next()
                        tt("dve", t1[:, :], pg[:, :], rA[:, 1, :], ALU.mult, [bG2, bRA], [b1])
                        tt("dve", t2[:, :], pf[:, :], rA[:, 0, :], ALU.mult, [bF, bRA], [b2])
                        tt("pool", ob[:, :], t1[:, :], t2[:, :], ALU.add, [b1, b2], [bOB])
                        if ct < 8:
                            pr2 = ct // 2
                            dst = QaTf[pr2 * 128:(pr2 + 1) * 128, c0:c0 + 512]
                        else:
                            br = (ct - 8) // 2
                            dst = (KcT if br == 0 else (KsTf if br == 1 else KwTf))[:, c0:c0 + 512]
                        P.dma("sp", dst, ob[:, :], reads=[bOB])
                    elif ct == 14:
                        ob, bOB = obr.next()
                        cp("act", ob[:], pf[:], [bF], [bOB])
                        P.dma("sp", VcT[:, c0:c0 + 512], ob[:], reads=[bOB])
                    elif ct < 18:
                        i = ct - 15
                        act(sq[:, i, :], pf[:], AF.Square, [bF], [bSQ])
                        cp("dve", cq[:, i, :], pf[:], [bF], [bCQ])
                        if ct == 16 or ct == 17:
                            pf2, bF2 = pfr.next()
                            ks = (0, 1) if ct == 16 else (2,)
                            for n, kk in enumerate(ks):
                                mm(pf2[:], ones[:], sq[:, kk, :], n == 0, n == len(ks) - 1, [bO, bSQ], [bF2])
                            rs, bRS = rsr.next()
                            rstd_of(rs[:], pf2[:], 128.0 * len(ks), [bF2], [bRS])
                            for kk in ks:
                                ob, bOB = obr.next()
                                tt("dve", ob[:], cq[:, kk, :], rs[:], ALU.mult, [bCQ, bRS], [bOB])
                                dst = cqnT[kk * 128:(kk + 1) * 128, c0:c0 + 512] if kk < 2 else ckvnT[:, c0:c0 + 512]
                                P.dma("sp", dst, ob[:], reads=[bOB])
                    else:
                        t1, b1 = t1r.next()
                        t2, b2 = t2r.next()
                        ob, bOB = obr.next()
                        tt("dve", t1[0:32, :], pf[0:32, :], rB[0:32, :], ALU.mult, [bF, bRB], [b1])
                        tt("dve", t2[0:32, :], pf[32:64, :], rB[32:64, :], ALU.mult, [bF, bRB], [b2])
                        tt("pool", ob[0:32, :], t1[0:32, :], t2[0:32, :], ALU.add, [b1, b2], [bOB])
                        P.dma("sp", krT[:, c0:c0 + 512], ob[0:32, :], reads=[bOB])
                for j in range(4):
                    t = tg * 4 + j
                    pm, bM = pmr.next()
                    for k in range(8):
                        mm(pm[:, 0:280], hT[:, k, j * 128:(j + 1) * 128], Wt[:, k, TM0:TM0 + 280], k == 0, k == 7,
                           [bW, bH], [bM])
                    tm, bT = tmr.next()
                    cp("act", tm[:, 0:256], pm[:, 0:256], [bM], [bT])
                    P.dma("sp", Vs[t * 128:(t + 1) * 128, :], tm[:, 0:128], reads=[bT])
                    P.dma("sp", Vw[t * 128:(t + 1) * 128, :], tm[:, 128:256], reads=[bT])
                    gt_, bG = gtr.next()
                    act(gt_[:], pm[:, 256:280], AF.Exp, [bM], [bG], scale=-1.0)
                    ts("dve", gt_[:], gt_[:], 1.0, None, ALU.add, None, [bG], [bG])
                    recip(gt_[:], gt_[:], [bG], [bG])
                    P.dma("sp", gates[t * 128:(t + 1) * 128, :], gt_[:], reads=[bG])

            normT(0)
            normT2(0)
            for tg in range(NG):
                if tg + 1 < NG:
                    normT(tg + 1)
                proj(tg)
                if tg + 1 < NG:
                    normT2(tg + 1)
            P.flush()

    def phase_B(l):
        with ExitStack() as st:
            srcs = []
            for (XT, W1b, W2b, Pb) in ((KcT, W1k_b, W2k_b, Pk_b), (VcT, W1v_b, W2v_b, Pv_b)):
                xt = sbt(st, [128, S], BF16); bx = Buf()
                P.dma("sp", xt[:], XT, writes=[bx])
                w1 = sbt(st, [128, 32, 256], BF16); b1 = Buf()
                src = W1b[l].rearrange("(l d) h -> d l h", d=64)
                P.dma("sp", w1[0:64], src, writes=[b1])
                P.dma("sp", w1[64:128], src, writes=[b1])
                w2 = sbt(st, [128, 2, 64], BF16); b2 = Buf()
                P.dma("sp", w2[:], W2b[l].rearrange("(c p) d -> p c d", p=128), writes=[b2])
                pp = sbt(st, [64, 32], BF16); bp = Buf()
                P.dma("sp", pp[:], Pb[l], writes=[bp])
                srcs.append((xt, bx, w1, b1, w2, b2, pp, bp))
            bias = sbt(st, [128, 8], F32); bB = Buf()
            hid = Ring([sbt(st, [128, 2, 256], BF16) for _ in range(2)])
            ob = Ring([sbt(st, [128, 256], BF16) for _ in range(3)])
            pr = Ring(PS[0:4]); pr.bufs = PB[0:4]
            for kv, (xt, bx, w1, b1, w2, b2, pp, bp) in enumerate(srcs):
                for c in range(2):
                    pb_, bPb = pr.next()
                    for li in range(32):
                        mm(pb_[:, 0:1], w1[0:64, li, c * 128:(c + 1) * 128], pp[:, li:li + 1], li == 0, li == 31,
                           [b1, bp], [bPb])
                    cp("dve", bias[:, kv * 2 + c:kv * 2 + c + 1], pb_[:, 0:1], [bPb], [bB])
                for g in range(2):
                    hd, bh = hid.next()
                    for c in range(2):
                        ph, bPh = pr.next()
                        for li in range(32):
                            mm(ph[:, 0:NCMP], w1[g * 64:(g + 1) * 64, li, c * 128:(c + 1) * 128],
                               xt[g * 64:(g + 1) * 64, li:li + 16 * (NCMP - 1) + 1:16], li == 0, li == 31,
                               [b1, bx], [bPh])
                        act(hd[:, c, 0:NCMP], ph[:, 0:NCMP], AF.Silu, [bPh, bB], [bh],
                            bias=bias[:, kv * 2 + c:kv * 2 + c + 1])
                    if kv == 0:
                        po, bPo = pr.next()
                        for c in range(2):
                            mm(po[0:64, 0:NCMP], w2[:, c, :], hd[:, c, 0:NCMP], c == 0, c == 1, [b2, bh], [bPo])
                        o_, bo = ob.next()
                        cp("dve", o_[0:64, 0:NCMP], po[0:64, 0:NCMP], [bPo], [bo])
                        P.dma("pool", KcmpT[g, :, 0:NCMP], o_[0:64, 0:NCMP], reads=[bo])
                    else:
                        for n0 in range(0, NCMP, 128):
                            cnt = min(128, NCMP - n0)
                            po, bPo = pr.next()
                            for c in range(2):
                                mm(po[0:cnt, 0:64], hd[:, c, n0:n0 + cnt], w2[:, c, :], c == 0, c == 1, [b2, bh], [bPo])
                            o_, bo = ob.next()
                            cp("dve", o_[0:cnt, 0:64], po[0:cnt, 0:64], [bPo], [bo])
                            P.dma("pool", Vcmp[n0:n0 + cnt, g, :], o_[0:cnt, 0:64], reads=[bo])
            P.flush()

    class AttnCtx:
        pass

    def attn_setup(st):
        a = AttnCtx()
        a.sr = Ring(PS[0:3]); a.sr.bufs = PB[0:3]
        a.orr = Ring(PS[3:5]); a.orr.bufs = PB[3:5]
        a.tr = Ring(PS[5:7]); a.tr.bufs = PB[5:7]
        a.pr = Ring([sbt(st, [128, 512], BF16) for _ in range(4)])
        a.otr = Ring([sbt(st, [65, 512], F32) for _ in range(3)])
        a.ident = sbt(st, [128, 128], BF16); a.bI = Buf()
        P.dma("sp", a.ident[:], cb["identB"], writes=[a.bI])
        a.identf = sbt(st, [128, 128], F32); a.bIf = Buf()
        P.dma("sp", a.identf[:], cin["identB"], writes=[a.bIf])
        a.triC = sbt(st, [128, 2048], BF16); a.bTC = Buf()
        P.dma("sp", a.triC[:], cb["triC"], writes=[a.bTC])
        a.triW = sbt(st, [128, 2048], BF16); a.bTW = Buf()
        P.dma("sp", a.triW[:], cb["triW"], writes=[a.bTW])
        a.smr = Ring([sbt(st, [128, 16], F32) for _ in range(6)])
        return a

    class Pipe:
        def __init__(self, a, L=2, PRE=6, DEFER=3):
            self.a = a
            self.L = L
            self.PRE = PRE
            self.DEFER = DEFER
            self.runs = []

        def add(self, tiles, scale, q_ap, q_bufs, facs_fn, acc_fn, pre=None, post=None):
            self.runs.append([tiles, scale, q_ap, q_bufs, facs_fn, acc_fn, pre, post])

        def emit(self):
            a = self.a
            runs = self.runs
            self.runs = []
            jobs = []
            firsts = []
            for r, run in enumerate(runs):
                firsts.append(len(jobs))
                for i in range(len(run[0])):
                    jobs.append((r, i))
            nj = len(jobs)
            pre_done = 0
            sc_state = {}
            acc_state = {}
            pending = []
            for idx in range(nj + self.L):
                if idx < nj:
                    while pre_done < len(runs) and firsts[pre_done] - self.PRE <= idx:
                        if runs[pre_done][6] is not None:
                            runs[pre_done][6]()
                        pre_done += 1
                    r, i = jobs[idx]
                    tiles, scale, q_ap, q_bufs = runs[r][0:4]
                    kT, kb, va, vb, m = tiles[i]
                    cnt = kT.shape[1]
                    s_, bS = a.sr.next()
                    mm(s_[0:cnt, :], kT, q_ap(0, 512), True, m is None, list(kb) + list(q_bufs), [bS])
                    if m is not None:
                        mm(s_[0:cnt, :], m[0], m[1], False, True, list(m[2]), [bS])
                    sc_state[idx] = (s_, bS, cnt)
                k = idx - self.L
                if k >= 0:
                    r, i = jobs[k]
                    tiles, scale = runs[r][0:2]
                    kT, kb, va, vb, m = tiles[i]
                    s_, bS, cnt = sc_state.pop(k)
                    if i == 0:
                        acc_state[r] = a.orr.next()
                    po, bPo = acc_state[r]
                    p_, bP = a.pr.next()
                    act(p_[0:cnt, :], s_[0:cnt, :], AF.Exp, [bS], [bP], scale=scale)
                    mm(po[0:65, :], va, p_[0:cnt, :], i == 0, i == len(tiles) - 1, list(vb) + [bP], [bPo])
                    if i == len(tiles) - 1:
                        del acc_state[r]
                        ot, bOT = a.otr.next()
                        cp("dve", ot[:, :], po[0:65, :], [bPo], [bOT])
                        pending.append([self.DEFER, (lambda ot=ot, bOT=bOT, r=r: attn_finish2(a, ot, bOT, runs[r][4], runs[r][5], runs[r][7]))])
                for pnd in pending:
                    pnd[0] -= 1
                while pending and pending[0][0] <= 0:
                    pending.pop(0)[1]()
            for pnd in pending:
                pnd[1]()

    def attn_finish2(a, ot, bOT, facs_fn, acc_fn, post):
        ptk, bPT = a.tr.next()
        for j in range(4):
            tr(ptk[:, j * 65:(j + 1) * 65], ot[0:65, j * 128:(j + 1) * 128], a.identf[0:65, 0:65], [bOT, a.bIf], [bPT])
        sm, bSM = a.smr.next()
        v = ptk[:, 0:260].rearrange("p (j c) -> p j c", c=65)
        ts("dve", sm[:, 0:4], v[:, :, 64], 1e-30, None, ALU.max, None, [bPT], [bSM])
        recip(sm[:, 4:8], sm[:, 0:4], [bSM], [bSM])
        fac = facs_fn(sm, bSM)
        for j in range(4):
            acc_fn(j, ptk[:, j * 65:j * 65 + 64], fac[:, j:j + 1], [bPT, bSM])
        if post is not None:
            post()

    def phase_C(l):
        sc = DK ** -0.5
        with ExitStack() as st:
            a = attn_setup(st)
            KsA = []
            KwS = []
            for g in range(G):
                t_ = sbt(st, [128, S], BF16); b_ = Buf()
                P.dma("sp", t_[0:64, :], KsT[g], writes=[b_])
                P.dma("sp", t_[64:128, :], cb["eind"], writes=[b_])
                KsA.append((t_, b_))
                t2_ = sbt(st, [64, S], BF16); b2_ = Buf()
                P.dma("sp", t2_[:], KwT[g], writes=[b2_])
                KwS.append((t2_, b2_))
            VsA = sbt(st, [128, NT, G, 65], BF16); bVs = Buf()
            VwA = sbt(st, [128, NT, G, 65], BF16); bVw = Buf()
            ms("pool", VsA[:], 1.0, [bVs])
            ms("pool", VwA[:], 1.0, [bVw])
            for g in range(G):
                P.dma("sp", VsA[:, :, g, 0:64], Vs[:, g * 64:(g + 1) * 64].rearrange("(t p) d -> p t d", p=128), writes=[bVs])
                P.dma("sp", VwA[:, :, g, 0:64], Vw[:, g * 64:(g + 1) * 64].rearrange("(t p) d -> p t d", p=128), writes=[bVw])
            KcS = sbt(st, [64, G, 256], BF16); bKc = Buf()
            P.dma("sp", KcS[:, :, 0:NCMP], KcmpT[:, :, 0:NCMP].rearrange("g d n -> d g n"), writes=[bKc])
            VcA = sbt(st, [128, 2, G, 65], BF16); bVc = Buf()
            ms("pool", VcA[:], 1.0, [bVc])
            for n0 in range(0, NCMP, 128):
                cnt = min(128, NCMP - n0)
                P.dma("sp", VcA[0:cnt, n0 // 128, :, 0:64], Vcmp[n0:n0 + cnt], writes=[bVc])
            gat = sbt(st, [128, NT, 24], F32); bGa = Buf()
            P.dma("sp", gat[:], gates.rearrange("(t p) c -> p t c", p=128), writes=[bGa])
            cmT = sbt(st, [33, 512], BF16); bCm = Buf()
            P.dma("sp", cmT[:], cb["cmT"], writes=[bCm])
            jb = sbt(st, [33, 400], BF16); bJb = Buf()
            P.dma("sp", jb[:], cb["jbig"], writes=[bJb])
            sv = sbt(st, [128, 128], F32); sa = sbt(st, [128, 128], F32); bSel = Buf()
            P.dma("sp", sv[:], selv, writes=[bSel])
            P.dma("sp", sa[:], sela, writes=[bSel])
            Qr = Ring([sbt(st, [128, NH, 512], BF16) for _ in range(2)])
            Qnb = [Buf(), Buf()]
            Er = Ring([sbt(st, [128, 256], F32) for _ in range(33)])
            smP = Ring([sbt(st, [128, 4], F32) for _ in range(33)])
            impr = Ring([sbt(st, [128, 264], F32) for _ in range(9)])
            wkr = Ring([sbt(st, [128, 320], F32) for _ in range(9)])
            accr = Ring([sbt(st, [128, 4, 512], F32) for _ in range(2)])
            fcr = Ring([sbt(st, [128, 8], F32) for _ in range(3)])
            mxr = Ring([sbt(st, [128, 4, 512], BF16) for _ in range(2)])
            junk = sbt(st, [128, 512], BF16); bJ = Buf()
            scr = Ring(PS[7:8]); scr.bufs = PB[7:8]
            pnb, bPnb = PS[7], PB[7]
            NBs = []
            for _ in range(8):
                t_ = sbt(st, [128, 128], BF16); b_ = Buf()
                ms("pool", t_[:], 0.0, [b_])
                NBs.append((t_, b_))
            Qs = {}
            wks = {}

            def load_q(qt):
                Q, bQ = Qr.next()
                bQn = Qnb[(Qr.i - 1) % 2]
                P.dma("sp", Q[0:64, :, :], QaT[:, :, qt * 512:(qt + 1) * 512].rearrange("h d s -> d h s"), writes=[bQ])
                Qs[qt] = (Q, bQ, bQn)

            def prelude1(qt, j, g):
                Q, bQ, bQn = Qs[qt]
                qi = qt * 4 + j
                ncnt = min(NCMP, 8 * qi + 7)
                imp, bImp = impr.next()
                ms("pool", imp[:], 0.0, [bImp])
                for r in range(4):
                    h = 4 * g + r
                    s_, bS = scr.next()
                    mm(s_[:, 0:ncnt], Q[0:64, h, j * 128:(j + 1) * 128], KcS[:, g, 0:ncnt], True, False, [bQ, bKc], [bS])
                    a0 = 258 - 8 * qi
                    mm(s_[:, 0:ncnt], cmT[0:9, 0:128], jb[0:9, a0:a0 + ncnt], False, True, [bCm, bJb], [bS])
                    E, bE = Er.next()
                    sm, bSM = smP.next()
                    act(E[:, 0:ncnt], s_[:, 0:ncnt], AF.Exp, [bS], [bE, bSM], scale=sc, accum=sm[:, 0:1])
                    ts("dve", sm[:, 1:2], sm[:, 0:1], 1e-30, None, ALU.max, None, [bSM], [bSM])
                    recip(sm[:, 2:3], sm[:, 1:2], [bSM], [bSM])
                    ts("pool", E[:, 0:ncnt], E[:, 0:ncnt], sm[:, 2:3], None, ALU.mult, None, [bE, bSM], [bE])
                    tt("pool", imp[:, 1:1 + ncnt], imp[:, 1:1 + ncnt], E[:, 0:ncnt], ALU.add, [bE, bImp], [bImp])
                wk, bWk = wkr.next()
                ib = wk[:, 0:64]
                nv = 4 * (NBLK - 1) + 1
                tt("pool", ib[:, 0:NBLK], imp[:, 1:1 + nv:4], imp[:, 2:2 + nv:4], ALU.add, [bImp], [bWk])
                tt("pool", ib[:, 0:NBLK], ib[:, 0:NBLK], imp[:, 3:3 + nv:4], ALU.add, [bImp, bWk], [bWk])
                ts("pool", ib[:, 0:NBLK], ib[:, 0:NBLK], 2.0, None, ALU.mult, None, [bWk], [bWk])
                tt("pool", ib[:, 0:NBLK], ib[:, 0:NBLK], imp[:, 0:nv:4], ALU.add, [bImp, bWk], [bWk])
                tt("pool", ib[:, 0:NBLK], ib[:, 0:NBLK], imp[:, 4:4 + nv:4], ALU.add, [bImp, bWk], [bWk])
                s0 = 62 - 2 * qi
                if NBLK < 64:
                    ms("pool", wk[:, NBLK:64], -1.0, [bWk])
                tt("pool", ib[:, 0:NBLK], ib[:, 0:NBLK], sv[:, s0:s0 + NBLK], ALU.mult, [bWk, bSel], [bWk])
                tt("pool", ib[:, 0:NBLK], ib[:, 0:NBLK], sa[:, s0:s0 + NBLK], ALU.add, [bWk, bSel], [bWk])
                ms("pool", wk[:, 0:1], 20000.0, [bWk])
                wks[(qt, j, g)] = (wk, bWk)

            def prelude1b(qt, j, g):
                wk, bWk = wks.pop((qt, j, g))
                P.op("dve", lambda e, o=wk[:, 64:72], i=wk[:, 0:64]: e.max(out=o, in_=i), [bWk], [bWk])
                P.op("dve", lambda e, o=wk[:, 128:192], r_=wk[:, 64:72], i=wk[:, 0:64]:
                     e.match_replace(out=o, in_to_replace=r_, in_values=i, imm_value=-1e30), [bWk], [bWk])
                P.op("dve", lambda e, o=wk[:, 72:80], i=wk[:, 128:192]: e.max(out=o, in_=i), [bWk], [bWk])
                ts("dve", wk[:, 192:256], wk[:, 0:64], wk[:, 79:80], -NEG, ALU.is_ge, ALU.mult, [bWk], [bWk])
                NBt, bNB = NBs[j * 2 + g]
                ts("dve", NBt[:, 64:128], wk[:, 192:256], NEG, None, ALU.add, None, [bWk], [bNB])

            def prelude2(qt, j, g):
                Q, bQ, bQn = Qs[qt]
                NBt, bNB = NBs[j * 2 + g]
                pnbb = pnb[:].bitcast(BF16)
                tr(pnbb[:, 0:128], NBt[:], a.ident[:], [bNB, a.bI], [bPnb])
                for r in range(4):
                    cp("act" if r % 2 else "dve", Q[64:128, 4 * g + r, j * 128:(j + 1) * 128],
                       pnbb[64:128, 0:128], [bPnb], [bQn])

            load_q(0)
            for j in range(4):
                for g in range(G):
                    prelude1(0, j, g)
            for j in range(4):
                for g in range(G):
                    prelude1b(0, j, g)
                    prelude2(0, j, g)
            for qt in range(NG):
                q0 = qt * 512
                Q, bQ, bQn = Qs[qt]
                nxt = qt + 1 < NG
                if nxt:
                    load_q(qt + 1)
                    for j in range(4):
                        for g in range(G):
                            prelude1(qt + 1, j, g)
                acc, bAcc = accr.next()
                pipe = Pipe(a)

                def add_run(h, bi, br, tiles, qfun, qb, pre):
                    fc, bFc = fcr.next()

                    def facs(sm, bSM, fc=fc, bFc=bFc, h=h, br=br, qt=qt):
                        tt("dve", fc[:, 0:4], sm[:, 4:8], gat[:, qt * 4:qt * 4 + 4, h * 3 + br], ALU.mult,
                           [bSM, bGa], [bFc])
                        return fc

                    def accf(j, otok, f, reads, acc=acc, bAcc=bAcc, h=h, bi=bi, bFc=bFc):
                        dst = acc[:, j, h * 64:(h + 1) * 64]
                        if bi == 0:
                            ts("dve", dst, otok, f, None, ALU.mult, None, reads + [bFc], [bAcc])
                        else:
                            stt("dve", dst, otok, f, dst, ALU.mult, ALU.add, reads + [bFc, bAcc], [bAcc])
                    pipe.add(tiles, sc, qfun, qb, facs, accf, pre=pre)

                for h in range(NH):
                    g = h // 4
                    qf = lambda lo, hi, Q=Q, h=h: Q[0:64, h, lo:hi]
                    ncq = min(NCMP, 32 * qt + 31)
                    tiles = []
                    for n0 in range(0, ncq, 128):
                        cnt = min(128, ncq - n0)
                        a0 = n0 - 32 * qt + 258
                        tiles.append((KcS[:, g, n0:n0 + cnt], [bKc], VcA[0:cnt, n0 // 128, g, :], [bVc],
                                      (jb[:, a0:a0 + cnt], cmT[:, :], [bJb, bCm])))
                    add_run(h, 0, 0, tiles, qf, [bQ], None)
                    tiles = []
                    for jp in range(4):
                        k0 = q0 - 512 + 128 * jp
                        if k0 < 0:
                            continue
                        tiles.append((KwS[g][0][:, k0:k0 + 128], [KwS[g][1]], VwA[:, k0 // 128, g, :], [bVw],
                                      (a.ident[:], a.triW[:, jp * 512:(jp + 1) * 512], [a.bI, a.bTW])))
                    for jn in range(4):
                        k0 = q0 + 128 * jn
                        tiles.append((KwS[g][0][:, k0:k0 + 128], [KwS[g][1]], VwA[:, k0 // 128, g, :], [bVw],
                                      (a.ident[:], a.triC[:, jn * 512:(jn + 1) * 512], [a.bI, a.bTC])))
                    add_run(h, 1, 2, tiles, qf, [bQ], None)
                for h in range(NH):
                    g = h // 4
                    qfa = lambda lo, hi, Q=Q, h=h: Q[:, h, lo:hi]
                    tiles = []
                    for kt in range(4 * qt + 4):
                        k0 = kt * 128
                        jn = kt - 4 * qt
                        if jn < 0:
                            tiles.append((KsA[g][0][:, k0:k0 + 128], [KsA[g][1]], VsA[:, kt, g, :], [bVs], None))
                        else:
                            tiles.append((KsA[g][0][:, k0:k0 + 128], [KsA[g][1]], VsA[:, kt, g, :], [bVs],
                                          (a.ident[:], a.triC[:, jn * 512:(jn + 1) * 512], [a.bI, a.bTC])))
                    def pre2(qt=qt, h=h):
                        prelude1b(qt + 1, h // 2, h % 2)
                    add_run(h, 2, 1, tiles, qfa, [bQ, bQn], pre2 if nxt else None)
                pipe.emit()
                if nxt:
                    for j in range(4):
                        for g in range(G):
                            prelude2(qt + 1, j, g)
                mx, bMx = mxr.next()
                for j in range(4):
                    sm, bSM = a.smr.next()
                    act(junk[:], acc[:, j, :], AF.Square, [bAcc], [bJ, bSM], accum=sm[:, 0:1])
                    rstd_of(sm[:, 1:2], sm[:, 0:1], 512.0, [bSM], [bSM])
                    ts("dve", mx[:, j, :], acc[:, j, :], sm[:, 1:2], None, ALU.mult, None, [bAcc, bSM], [bMx])
                P.dma("pool", mixed[q0:q0 + 512, 0:512].rearrange("(j p) c -> p j c", p=128), mx[:], reads=[bMx])
            P.flush()

    def phase_D(l):
        sc = 96.0 ** -0.5
        with ExitStack() as st:
            a = attn_setup(st)
            ckv = sbt(st, [128, S], BF16); bCkv = Buf()
            P.dma("sp", ckv[:], ckvnT, writes=[bCkv])
            cqn = sbt(st, [128, 2, S], BF16); bCq = Buf()
            P.dma("sp", cqn[:], cqnT.rearrange("(c p) s -> p c s", p=128), writes=[bCq])
            wqt = sbt(st, [128, 2, 1024], BF16); bWq = Buf()
            P.dma("sp", wqt[:], Wq_b[l].rearrange("(c p) n -> p c n", p=128), writes=[bWq])
            wkt = sbt(st, [128, 1024], BF16); bWk = Buf()
            P.dma("sp", wkt[:], Wkv_b[l], writes=[bWk])
            VA = sbt(st, [128, NT, NH, 65], BF16); bVA = Buf()
            ms("pool", VA[:], 1.0, [bVA])
            rBr = Ring([sbt(st, [128, 512], F32) for _ in range(2)])
            bObD = Buf()
            pvr = Ring(PS[5:7]); pvr.bufs = PB[5:7]
            for t in range(NT):
                pv, bPv = pvr.next()
                mm(pv[:], ckv[:, t * 128:(t + 1) * 128], wkt[:, 512:1024], True, True, [bCkv, bWk], [bPv])
                cp("act" if t % 2 else "dve", VA[:, t, :, 0:64], pv[:].rearrange("p (h d) -> p h d", h=NH), [bPv], [bVA])
            Kr = Ring([sbt(st, [128, S], BF16) for _ in range(2)])
            Qr = Ring([sbt(st, [128, 512], BF16) for _ in range(4)])
            t1r = Ring([sbt(st, [128, 512], F32) for _ in range(2)])
            t2r = Ring([sbt(st, [128, 512], F32) for _ in range(2)])
            accr = Ring([sbt(st, [128, 4, 512], F32) for _ in range(2)])
            osr = Ring([sbt(st, [128, 4, 64], F32) for _ in range(5)])
            junk = sbt(st, [128, 512], BF16); bJ = Buf()
            mxr = Ring([sbt(st, [128, 4, 512], BF16) for _ in range(2)])
            pkr = Ring(PS[5:8]); pkr.bufs = PB[5:8]
            Ks = {}

            def build_k(h):
                K, bK = Kr.next()
                P.dma("sp", K[64:96, :], krT, writes=[bK])
                for tg in range(NG):
                    pk, bPk = pkr.next()
                    mm(pk[0:64, :], wkt[:, h * 64:(h + 1) * 64], ckv[:, tg * 512:(tg + 1) * 512], True, True, [bWk, bCkv], [bPk])
                    cp("dve", K[0:64, tg * 512:(tg + 1) * 512], pk[0:64, :], [bPk], [bK])
                Ks[h] = (K, bK)

            def build_q(h, qt, Q, bQ):
                q0 = qt * 512
                rBt, bRB = rBr.next()
                P.dma("sp", rBt[:], ropeB[:, q0:q0 + 512], writes=[bRB])
                pq, bPq = pkr.next()
                for c in range(2):
                    mm(pq[0:96, :], wqt[:, c, h * 128:h * 128 + 96], cqn[:, c, q0:q0 + 512], c == 0, c == 1, [bWq, bCq], [bPq])
                pr_, bPr = pkr.next()
                for c in range(2):
                    mm(pr_[0:32, :], wqt[:, c, h * 128 + 96:h * 128 + 128], cqn[:, c, q0:q0 + 512], c == 0, c == 1,
                       [bWq, bCq], [bPr])
                cp("dve", Q[0:64, :], pq[0:64, :], [bPq], [bQ])
                t1, b1 = t1r.next()
                t2, b2 = t2r.next()
                tt("dve", t1[64:96, :], pr_[0:32, :], rBt[0:32, :], ALU.mult, [bPr, bRB], [b1])
                tt("dve", t2[64:96, :], pq[64:96, :], rBt[64:96, :], ALU.mult, [bPq, bRB], [b2])
                tt("pool", Q[64:96, :], t1[64:96, :], t2[64:96, :], ALU.add, [b1, b2], [bQ])

            build_k(0)
            for h in range(NH):
                K, bK = Ks[h]
                pipe = Pipe(a, PRE=10)
                for qt in range(NG):
                    q0 = qt * 512
                    Q, bQ = Qr.next()
                    tiles = []
                    for kt in range(4 * qt + 4):
                        k0 = kt * 128
                        jn = kt - 4 * qt
                        if jn < 0:
                            tiles.append((K[0:96, k0:k0 + 128], [bK], VA[:, kt, h, :], [bVA], None))
                        else:
                            tiles.append((K[0:96, k0:k0 + 128], [bK], VA[:, kt, h, :], [bVA],
                                          (a.ident[:], a.triC[:, jn * 512:(jn + 1) * 512], [a.bI, a.bTC])))
                    os_, bOs = osr.next()

                    def facs(sm, bSM):
                        return sm[:, 4:8]

                    def accf(j, otok, f, reads, os_=os_, bOs=bOs):
                        ts("dve", os_[:, j, :], otok, f, None, ALU.mult, None, reads, [bOs])

                    def post(os_=os_, bOs=bOs, q0=q0, h=h):
                        P.dma("pool", obD[q0:q0 + 512, h * 64:(h + 1) * 64].rearrange("(j p) c -> p j c", p=128), os_[:],
                              reads=[bOs], writes=[bObD])

                    def pre(h=h, qt=qt, Q=Q, bQ=bQ):
                        build_q(h, qt, Q, bQ)
                        if qt == NG - 1 and h + 1 < NH:
                            build_k(h + 1)
                    pipe.add(tiles, sc, (lambda lo, hi, Q=Q: Q[0:96, lo:hi]), [bQ], facs, accf, pre=pre, post=post)
                pipe.emit()
            for qt in range(NG):
                mx, bMx = mxr.next()
                acc, bAcc = accr.next()
                P.dma("sp", acc[:], obD[qt * 512:(qt + 1) * 512, :].rearrange("(j p) c -> p j c", p=128),
                      reads=[bObD], writes=[bAcc])
                for j in range(4):
                    sm, bSM = a.smr.next()
                    act(junk[:], acc[:, j, :], AF.Square, [bAcc], [bJ, bSM], accum=sm[:, 0:1])
                    rstd_of(sm[:, 1:2], sm[:, 0:1], 512.0, [bSM], [bSM])
                    ts("dve", mx[:, j, :], acc[:, j, :], sm[:, 1:2], None, ALU.mult, None, [bAcc, bSM], [bMx])
                P.dma("pool", mixed[qt * 512:(qt + 1) * 512, 512:1024].rearrange("(j p) c -> p j c", p=128), mx[:],
                      reads=[bMx])
            P.flush()

    def phase_E1(l):
        xsrc = x_in if l == 0 else xs
        with ExitStack() as st:
            wo = sbt(st, [128, 8, D], BF16); bWo = Buf()
            P.dma("sp", wo[:], Wout_b[l].rearrange("(k p) n -> p k n", p=128), writes=[bWo])
            ident = sbt(st, [128, 128], BF16); bI = Buf()
            P.dma("sp", ident[:], cb["identB"], writes=[bI])
            mr = Ring([sbt(st, [128, D], BF16) for _ in range(3)])
            mTr = Ring([sbt(st, [128, 8, 128], BF16) for _ in range(3)])
            xr = Ring([sbt(st, [128, D], F32) for _ in range(4)])
            xnr = Ring([sbt(st, [128, D], F32) for _ in range(3)])
            hr = Ring([sbt(st, [128, D], BF16) for _ in range(4)])
            hTr = Ring([sbt(st, [128, 8, 128], BF16) for _ in range(3)])
            statr = Ring([sbt(st, [128, 4], F32) for _ in range(3)])
            junk = sbt(st, [128, D], BF16); bJ = Buf()
            ptr = Ring(PS[0:2]); ptr.bufs = PB[0:2]
            por = Ring([(PS[2], PS[3]), (PS[4], PS[5])]); por.bufs = [(PB[2], PB[3]), (PB[4], PB[5])]
            pt2 = Ring(PS[6:8]); pt2.bufs = PB[6:8]
            st1 = {}
            st2 = {}

            def s1(t):
                m_, bM = mr.next()
                P.dma("sp", m_[:], mixed[t * 128:(t + 1) * 128, :], writes=[bM])
                xt, bX = xr.next()
                P.dma("sp", xt[:], xsrc[t * 128:(t + 1) * 128, :], writes=[bX])
                pt, bP = ptr.next()
                ptb = pt[:].bitcast(BF16)
                for k in range(8):
                    tr(ptb[:, k * 128:(k + 1) * 128], m_[:, k * 128:(k + 1) * 128], ident[:], [bM, bI], [bP])
                mT, bMT = mTr.next()
                cp("act", mT[:], ptb[:, 0:1024].rearrange("p (k t) -> p k t", k=8), [bP], [bMT])
                st1[t] = (xt, bX, mT, bMT)

            def s2(t):
                xt, bX, mT, bMT = st1.pop(t)
                (p0, p1), (b0, b1) = por.next()
                xn, bXN = xnr.next()
                for half, (pp, bb) in enumerate(((p0, b0), (p1, b1))):
                    for k in range(8):
                        mm(pp[:], mT[:, k, :], wo[:, k, half * 512:(half + 1) * 512], k == 0, k == 7, [bMT, bWo], [bb])
                    tt("dve", xn[:, half * 512:(half + 1) * 512], pp[:], xt[:, half * 512:(half + 1) * 512], ALU.add,
                       [bb, bX], [bXN])
                P.dma("pool", xs[t * 128:(t + 1) * 128, :], xn[:], reads=[bXN])
                sx, bS = statr.next()
                act(junk[:], xn[:], AF.Square, [bXN], [bJ, bS], accum=sx[:, 0:1])
                rstd_of(sx[:, 1:2], sx[:, 0:1], D, [bS], [bS])
                hh, bHH = hr.next()
                ts("dve", hh[:], xn[:], sx[:, 1:2], None, ALU.mult, None, [bXN, bS], [bHH])
                st2[t] = (hh, bHH)

            def s3(t):
                hh, bHH = st2.pop(t)
                pq, bQ = pt2.next()
                pqb = pq[:].bitcast(BF16)
                for k in range(8):
                    tr(pqb[:, k * 128:(k + 1) * 128], hh[:, k * 128:(k + 1) * 128], ident[:], [bHH, bI], [bQ])
                hT, bHT = hTr.next()
                cp("act", hT[:], pqb[:, 0:1024].rearrange("p (k t) -> p k t", k=8), [bQ], [bHT])
                P.dma("pool", h2T[:, t * 128:(t + 1) * 128].rearrange("(k p) t -> p k t", p=128), hT[:], reads=[bHT])

            for t in range(NT + 3):
                if t < NT:
                    s1(t)
                if 0 <= t - 1 < NT:
                    s2(t - 1)
                if 0 <= t - 3 < NT:
                    s3(t - 3)
            P.flush()

    def phase_E2(l, last):
        with ExitStack() as st:
            w2 = sbt(st, [128, 32, D], BF16); bW2 = Buf()
            for c4 in range(4):
                P.dma("sp", w2[:, c4 * 8:(c4 + 1) * 8, :],
                      W2_b[l, c4 * 1024:(c4 + 1) * 1024, :].rearrange("(c p) n -> p c n", p=128), writes=[bW2])
            w1r = Ring([sbt(st, [128, 8, 512], BF16) for _ in range(2)])
            hTr = Ring([sbt(st, [128, 8, 512], BF16) for _ in range(2)])
            hdr = Ring([sbt(st, [128, 32, 512], BF16) for _ in range(1)])
            rr = Ring([sbt(st, [128, 512], F32) for _ in range(3)])
            xr = Ring([sbt(st, [128, D], F32) for _ in range(2)])
            xnr = Ring([sbt(st, [128, D], F32) for _ in range(2)])
            statr = Ring([sbt(st, [128, 4], F32) for _ in range(3)])
            junk = sbt(st, [128, D], BF16); bJ = Buf()
            gf = sbt(st, [128, D], F32); bGf = Buf()
            if last:
                P.dma("sp", gf[:], g_fin, writes=[bGf])
            p1r = Ring(PS[0:4]); p1r.bufs = PB[0:4]
            por = Ring([(PS[4], PS[5]), (PS[6], PS[7])]); por.bufs = [(PB[4], PB[5]), (PB[6], PB[7])]
            for tg in range(NG):
                c0 = tg * 512
                hT, bH = hTr.next()
                P.dma("sp", hT[:], h2T[:, c0:c0 + 512].rearrange("(k p) t -> p k t", p=128), writes=[bH])
                hd, bHd = hdr.next()
                for c4 in range(8):
                    w1, bW1 = w1r.next()
                    P.dma("sp", w1[:], W1_b[l, :, c4 * 512:(c4 + 1) * 512].rearrange("(k p) n -> p k n", p=128), writes=[bW1])
                    for cc in range(4):
                        c = c4 * 4 + cc
                        p1, bP1 = p1r.next()
                        for k in range(8):
                            mm(p1[:], w1[:, k, cc * 128:(cc + 1) * 128], hT[:, k, :], k == 0, k == 7, [bW1, bH], [bP1])
                        r_, bR = rr.next()
                        act(r_[:], p1[:], AF.Relu, [bP1], [bR])
                        tt("pool", hd[:, c, :], r_[:], r_[:], ALU.mult, [bR], [bHd])
                for j in range(4):
                    t = tg * 4 + j
                    xt, bX = xr.next()
                    P.dma("sp", xt[:], xs[t * 128:(t + 1) * 128, :], writes=[bX])
                    (p0, p1_), (b0, b1) = por.next()
                    xn, bXN = xnr.next()
                    for half, (pp, bb) in enumerate(((p0, b0), (p1_, b1))):
                        for c in range(32):
                            mm(pp[:], hd[:, c, j * 128:(j + 1) * 128], w2[:, c, half * 512:(half + 1) * 512],
                               c == 0, c == 31, [bHd, bW2], [bb])
                        tt("dve", xn[:, half * 512:(half + 1) * 512], pp[:], xt[:, half * 512:(half + 1) * 512], ALU.add,
                           [bb, bX], [bXN])
                    if not last:
                        P.dma("pool", xs[t * 128:(t + 1) * 128, :], xn[:], reads=[bXN], writes=[])
                    else:
                        sx, bS = statr.next()
                        act(junk[:], xn[:], AF.Square, [bXN], [bJ, bS], accum=sx[:, 0:1])
                        rstd_of(sx[:, 1:2], sx[:, 0:1], D, [bS], [bS])
                        stt("dve", xn[:], xn[:], sx[:, 1:2], gf[:], ALU.mult, ALU.mult, [bXN, bS, bGf], [bXN])
                        P.dma("pool", out[t * 128:(t + 1) * 128, :], xn[:], reads=[bXN])
            P.flush()

    phase_W()
    for l in range(DEPTH):
        phase_A(l)
        phase_B(l)
        phase_C(l)
        phase_D(l)
        phase_E1(l)
        phase_E2(l, l == DEPTH - 1)
    top.close()
    nc._n_emitted = P.ninst
    return nc


def host_inputs(inp, S, DEPTH):
    f = lambda a: np.ascontiguousarray(np.asarray(a, dtype=np.float32))
    w_in = f(inp["w_in"])[:DEPTH]
    w_in = np.concatenate([w_in, np.zeros((DEPTH, D, 1), np.float32)], axis=2)[:, :, _win_cols()]
    ropeA, ropeB = _rope_tables(S)
    shared = {
        "w_in": np.ascontiguousarray(w_in),
        "g_attn": _pk(f(inp["attn_norm"])[:DEPTH]),
        "w1k": f(inp["cmp_w1_k"])[:DEPTH], "w1v": f(inp["cmp_w1_v"])[:DEPTH],
        "w2k": f(inp["cmp_w2_k"])[:DEPTH], "w2v": f(inp["cmp_w2_v"])[:DEPTH],
        "posk": np.ascontiguousarray(f(inp["cmp_pos_k"])[:DEPTH].transpose(0, 2, 1)),
        "posv": np.ascontiguousarray(f(inp["cmp_pos_v"])[:DEPTH].transpose(0, 2, 1)),
        "wq": np.ascontiguousarray(f(inp["w_q_up"])[:DEPTH][:, :, _wq_cols()]),
        "g_q": _pk(f(inp["mla_q_norm"])[:DEPTH]),
        "wkv": np.ascontiguousarray(f(inp["w_kv_up"])[:DEPTH][:, :, _wkv_cols()]),
        "g_kv": _pk(f(inp["mla_kv_norm"])[:DEPTH]),
        "w_out": f(inp["w_out"])[:DEPTH],
        "g_mix": _pk(np.concatenate([f(inp["nsa_out_norm"])[:DEPTH], f(inp["mla_out_norm"])[:DEPTH]], axis=1)),
        "w_ff1": f(inp["w_ff1"])[:DEPTH],
        "g_mlp": _pk(f(inp["mlp_norm"])[:DEPTH]),
        "w_ff2": f(inp["w_ff2"])[:DEPTH],
        "g_fin": np.ascontiguousarray(np.tile(f(inp["final_norm"]).reshape(1, D), (128, 1))),
        "ropeA": ropeA, "ropeB": ropeB,
    }
    for k, v in _consts(S).items():
        shared["c_" + k] = v
    return shared


_CACHE = {}


def kernel(**inputs):
    x = np.asarray(inputs["x"], dtype=np.float32)
    B, S, _ = x.shape
    DEPTH = np.asarray(inputs["w_in"]).shape[0]
    key = (S, DEPTH)
    if key not in _CACHE:
        _CACHE[key] = build(S, DEPTH)
    nc = _CACHE[key]
    shared = host_inputs(inputs, S, DEPTH)
    in_maps = []
    for b in range(B):
        m = dict(shared)
        m["x"] = np.ascontiguousarray(x[b])
        in_maps.append(m)
    res = run_bass_kernel_spmd(nc, in_maps, core_ids=list(range(B)))
    return np.stack([np.asarray(r["out"], dtype=np.float32) for r in res.results], axis=0)
```

```python
import numpy as np
from contextlib import ExitStack
import concourse.bass as bass
import concourse.mybir as mybir
from concourse.bass_utils import run_bass_kernel_spmd

F32 = mybir.dt.float32
BF16 = mybir.dt.bfloat16
ALU = mybir.AluOpType
AF = mybir.ActivationFunctionType
AX = mybir.AxisListType

D = 1024
NH = 8
G = 2
DK = 64
HID = 4096
RQ = 256
RKV = 128
NEG = -30000.0
EPS = 1e-6
WINC = 19 * 128 + 280
TM0 = 19 * 128


class Buf:
    __slots__ = ("name", "w", "r", "psum")

    def __init__(self, name="", psum=False):
        self.name = name
        self.w = None
        self.r = []
        self.psum = psum


class Op:
    __slots__ = ("eng", "fn", "deps", "dma", "flag", "sem", "val")

    def __init__(self, eng, fn, dma):
        self.eng = eng
        self.fn = fn
        self.dma = dma
        self.deps = []
        self.flag = False
        self.sem = None
        self.val = 0


class Prog:
    ENGS = ("pe", "act", "dve", "pool", "sp")
    NDS = 12

    def __init__(self, nc, es):
        self.nc = nc
        self.ops = {e: [] for e in self.ENGS}
        self.esem = {e: es.enter_context(nc.semaphore("s_" + e)) for e in ("pe", "act", "dve", "pool")}
        self.dsem = {e: [es.enter_context(nc.semaphore("d_%s%d" % (e, i))) for i in range(self.NDS)]
                     for e in ("sp", "pool")}
        self.ecnt = {e: 0 for e in self.ENGS}
        self.dk = {e: 0 for e in self.ENGS}
        self.dcount = {e: [0] * self.NDS for e in ("sp", "pool")}
        self.known = {e: {} for e in self.ENGS}
        self.ninst = 0
        self.ecnt_done = {}

    def op(self, eng, fn, reads=(), writes=(), dma=False):
        o = Op(eng, fn, dma)
        deps = {}
        for b in reads:
            if b.w is not None:
                deps[id(b.w)] = (b.w, True)
            if b.psum:
                for r in b.r:
                    if r.eng != eng and id(r) not in deps:
                        deps[id(r)] = (r, False)
        for b in writes:
            if b.w is not None and id(b.w) not in deps:
                deps[id(b.w)] = (b.w, False)
            for r in b.r:
                if id(r) not in deps:
                    deps[id(r)] = (r, False)
        for d, raw in deps.values():
            if d is o:
                continue
            if d.dma or dma:
                o.deps.append(d)
            elif d.eng == eng:
                if eng != "pe":
                    o.deps.append(d)
            else:
                o.deps.append(d)
        for b in reads:
            b.r.append(o)
        for b in writes:
            b.w = o
            b.r = []
        self.ops[eng].append(o)
        return o

    def dma(self, eng, out, in_, reads=(), writes=()):
        return self.op(eng, lambda e: e.dma_start(out=out, in_=in_), reads, writes, dma=True)

    def flush(self):
        nc = self.nc
        lasts = []
        for e in self.ENGS:
            for o in reversed(self.ops[e]):
                if not o.dma and o.fn is not None:
                    lasts.append(o)
                    break
        alld = [o for e in self.ENGS for o in self.ops[e] if o.dma]
        for e in self.ENGS:
            b = Op(e, None, False)
            b.deps = list(lasts) + alld
            self.ops[e].append(b)
        for e in self.ENGS:
            for o in self.ops[e]:
                for d in o.deps:
                    d.flag = True
        for e in self.ENGS:
            for o in self.ops[e]:
                if o.dma:
                    s = self.dk[e] % self.NDS
                    self.dk[e] += 1
                    self.dcount[e][s] += 16
                    o.sem = self.dsem[e][s]
                    o.val = self.dcount[e][s]
                    o.flag = True
                elif o.flag and o.fn is not None:
                    self.ecnt[e] += 1
                    o.sem = self.esem[e]
                    o.val = self.ecnt[e]
        with nc.Block() as block:
            engobj = {"pe": block.tensor, "act": block.scalar, "dve": block.vector,
                      "pool": block.gpsimd, "sp": block.sync}

            def make_body(e):
                ops = self.ops[e]
                known = self.known[e]

                def body(eng):
                    emitted = [self.ecnt_done.get(e, 0)]
                    for o in ops:
                        need = {}
                        for d in o.deps:
                            if d.sem is None:
                                continue
                            key = id(d.sem)
                            if known.get(key, 0) >= d.val:
                                continue
                            if key not in need or need[key][1] < d.val:
                                need[key] = (d.sem, d.val)
                        if o.dma:
                            key = id(o.sem)
                            pv = o.val - 16
                            if pv > 0 and known.get(key, 0) < pv:
                                if key not in need or need[key][1] < pv:
                                    need[key] = (o.sem, pv)
                        for key, (s, v) in need.items():
                            if e in self.esem and s is self.esem[e]:
                                v = max(v, emitted[0] - 2)
                            eng.wait_ge(s, v)
                            known[key] = v
                            self.ninst += 1
                        if o.fn is None:
                            continue
                        ins = o.fn(eng)
                        self.ninst += 1
                        if o.flag:
                            ins.then_inc(o.sem, 16 if o.dma else 1)
                            if not o.dma:
                                emitted[0] = o.val
                return body

            for e in self.ENGS:
                if self.ops[e]:
                    engobj[e](make_body(e))
        self.ecnt_done = dict(self.ecnt)
        self.ops = {e: [] for e in self.ENGS}


class Ring:
    def __init__(self, tiles):
        self.tiles = tiles
        self.bufs = [Buf() for _ in tiles]
        self.i = 0

    def next(self):
        k = self.i % len(self.tiles)
        self.i += 1
        return self.tiles[k], self.bufs[k]


def _rope_tables(S):
    t = np.arange(S, dtype=np.float32)[:, None]
    invA = (1.0 / (np.float32(10000.0) ** (np.arange(0, 64, 2, dtype=np.float32) / np.float32(64)))).astype(np.float32)
    angA = (t * invA[None, :]).astype(np.float32)
    cosA = np.cos(angA).astype(np.float32)
    sinA = np.sin(angA).astype(np.float32)
    ropeA = np.zeros((2, 128, S), np.float32)
    for r0 in (0, 64):
        ropeA[0, r0:r0 + 32] = cosA.T
        ropeA[0, r0 + 32:r0 + 64] = cosA.T
        ropeA[1, r0:r0 + 32] = -sinA.T
        ropeA[1, r0 + 32:r0 + 64] = sinA.T
    invB = (1.0 / (np.float32(10000.0) ** (np.arange(0, 32, 2, dtype=np.float32) / np.float32(32)))).astype(np.float32)
    angB = (t * invB[None, :]).astype(np.float32)
    cosB = np.cos(angB).astype(np.float32)
    sinB = np.sin(angB).astype(np.float32)
    ropeB = np.zeros((128, S), np.float32)
    ropeB[0:16] = -sinB.T
    ropeB[16:32] = sinB.T
    ropeB[32:48] = cosB.T
    ropeB[48:64] = cosB.T
    ropeB[64:80] = cosB.T
    ropeB[80:96] = cosB.T
    return ropeA, ropeB


def _consts(S):
    c = {}
    c["identB"] = np.eye(128, dtype=np.float32)
    k = np.arange(128)[:, None]
    q = np.arange(128)[None, :]
    q5 = np.arange(512)[None, :]
    c["triC"] = np.concatenate([np.where(128 * j + k <= q5, 0.0, NEG) for j in range(4)], axis=1).astype(np.float32)
    c["triW"] = np.concatenate([np.where(q5 < 128 * j + k, 0.0, NEG) for j in range(4)], axis=1).astype(np.float32)
    c["eind"] = (np.arange(S)[None, :] // 64 == np.arange(64)[:, None]).astype(np.float32)
    i = np.arange(33)[:, None]
    ql = np.arange(512)[None, :]
    c["cmT"] = np.where(16 * (i - 2) + 31 <= ql, 0.0, NEG).astype(np.float32)
    c["jbig"] = (np.arange(400)[None, :] == i + 256).astype(np.float32)
    p = np.arange(128)[:, None]
    cc = np.arange(128)[None, :]
    d = cc - 62
    hi = (p >= 64).astype(np.int64)
    valid = d <= hi
    cur = d == hi
    prev = d == hi - 1
    forced = cur | prev
    c["selv"] = (valid & ~forced).astype(np.float32)
    c["sela"] = np.where(cur, 10000.0, np.where(prev, 10001.0, np.where(valid, 0.0, -1.0))).astype(np.float32)
    return c


def _win_cols():
    idx = []
    rot = lambda b: list(range(b + 32, b + 64)) + list(range(b, b + 32))
    for p in range(4):
        b0, b1 = 64 * (2 * p), 64 * (2 * p + 1)
        idx += list(range(b0, b0 + 64)) + list(range(b1, b1 + 64))
        idx += rot(b0) + rot(b1)
    for kb in (512, 768, 1024):
        idx += list(range(kb, kb + 128))
        idx += rot(kb) + rot(kb + 64)
    idx += list(range(640, 768))
    idx += list(range(1304, 1560))
    idx += list(range(1560, 1688))
    b = 1688
    idx += list(range(b + 16, b + 32)) + list(range(b, b + 16)) + list(range(b, b + 32)) + [1720] * 64
    idx += list(range(896, 1024)) + list(range(1152, 1280)) + list(range(1280, 1304))
    assert len(idx) == WINC
    return np.array(idx)


def _wq_cols():
    idx = []
    for h in range(8):
        b = 96 * h
        idx += list(range(b, b + 96)) + list(range(b + 80, b + 96)) + list(range(b + 64, b + 80))
    return np.array(idx)


def _wkv_cols():
    idx = []
    for h in range(8):
        idx += list(range(128 * h, 128 * h + 64))
    for h in range(8):
        idx += list(range(128 * h + 64, 128 * h + 128))
    return np.array(idx)


def _pk(g):
    L, n = g.shape
    return np.ascontiguousarray(g.reshape(L, n // 128, 128).transpose(0, 2, 1))


def build(S, DEPTH):
    NT = S // 128
    NG = S // 512
    NCMP = S // 16 - 1
    NBLK = S // 64
    nc = bass.Bass("TRN2", target_bir_lowering=False)

    def din(name, shape, dt=F32):
        return nc.dram_tensor(name, list(shape), dt, kind="ExternalInput").ap()

    def dsc(name, shape, dt=BF16):
        return nc.dram_tensor(name, list(shape), dt).ap()

    x_in = din("x", [S, D])
    w_in = din("w_in", [DEPTH, D, WINC])
    g_attn = din("g_attn", [DEPTH, 128, 8])
    w1k = din("w1k", [DEPTH, 2048, 256])
    w1v = din("w1v", [DEPTH, 2048, 256])
    w2k = din("w2k", [DEPTH, 256, 64])
    w2v = din("w2v", [DEPTH, 256, 64])
    posk = din("posk", [DEPTH, 64, 32])
    posv = din("posv", [DEPTH, 64, 32])
    wq = din("wq", [DEPTH, RQ, 1024])
    g_q = din("g_q", [DEPTH, 128, 2])
    wkv = din("wkv", [DEPTH, RKV, 1024])
    g_kv = din("g_kv", [DEPTH, 128, 1])
    w_out = din("w_out", [DEPTH, D, D])
    g_mix = din("g_mix", [DEPTH, 128, 8])
    w_ff1 = din("w_ff1", [DEPTH, D, HID])
    g_mlp = din("g_mlp", [DEPTH, 128, 8])
    w_ff2 = din("w_ff2", [DEPTH, HID, D])
    g_fin = din("g_fin", [128, D])
    ropeA = din("ropeA", [2, 128, S])
    ropeB = din("ropeB", [128, S])
    cshape = {"identB": [128, 128], "triC": [128, 2048], "triW": [128, 2048], "eind": [64, S],
              "cmT": [33, 512], "jbig": [33, 400]}
    cin = {k: din("c_" + k, v) for k, v in cshape.items()}
    selv = din("c_selv", [128, 128])
    sela = din("c_sela", [128, 128])
    out = nc.dram_tensor("out", [S, D], F32, kind="ExternalOutput").ap()

    cb = {k: dsc("b_" + k, v) for k, v in cshape.items()}
    Win_b = dsc("Win_b", [DEPTH, D, WINC])
    W1k_b = dsc("W1k_b", [DEPTH, 2048, 256])
    W1v_b = dsc("W1v_b", [DEPTH, 2048, 256])
    W2k_b = dsc("W2k_b", [DEPTH, 256, 64])
    W2v_b = dsc("W2v_b", [DEPTH, 256, 64])
    Pk_b = dsc("Pk_b", [DEPTH, 64, 32])
    Pv_b = dsc("Pv_b", [DEPTH, 64, 32])
    Wq_b = dsc("Wq_b", [DEPTH, RQ, 1024])
    Wkv_b = dsc("Wkv_b", [DEPTH, RKV, 1024])
    Wout_b = dsc("Wout_b", [DEPTH, D, D])
    W1_b = dsc("W1_b", [DEPTH, D, HID])
    W2_b = dsc("W2_b", [DEPTH, HID, D])
    xs = dsc("xs", [S, D], F32)
    QaT = dsc("QaT", [NH, 64, S])
    KcT = dsc("KcT", [128, S])
    VcT = dsc("VcT", [128, S])
    KsT = dsc("KsT", [G, 64, S])
    KwT = dsc("KwT", [G, 64, S])
    Vs = dsc("Vs", [S, 128])
    Vw = dsc("Vw", [S, 128])
    gates = dsc("gates", [S, 24], F32)
    cqnT = dsc("cqnT", [RQ, S])
    ckvnT = dsc("ckvnT", [RKV, S])
    krT = dsc("krT", [32, S])
    KcmpT = dsc("KcmpT", [G, 64, 256])
    Vcmp = dsc("Vcmp", [256, G, 64])
    mixed = dsc("mixed", [S, D])
    h2T = dsc("h2T", [D, S])
    obD = dsc("obD", [S, 512], F32)

    top = ExitStack()
    P = Prog(nc, top)
    PS = [top.enter_context(nc.psum_tensor("ps%d" % i, [128, 512], F32)) for i in range(8)]
    PB = [Buf("ps%d" % i, psum=True) for i in range(8)]
    uid = [0]

    def sbt(st, shape, dt=F32):
        uid[0] += 1
        return st.enter_context(nc.sbuf_tensor("t%d" % uid[0], list(shape), dt))

    def mm(o, l, r, start, stop, reads, writes):
        return P.op("pe", lambda e: e.matmul(o, lhsT=l, rhs=r, start=start, stop=stop), reads, writes)

    def tr(o, i, idn, reads, writes):
        return P.op("pe", lambda e: e.transpose(out=o, in_=i, identity=idn), reads, writes)

    def act(o, i, func, reads, writes, scale=1.0, bias=0.0, accum=None):
        if accum is None:
            return P.op("act", lambda e: e.activation(out=o, in_=i, func=func, bias=bias, scale=scale), reads, writes)
        return P.op("act", lambda e: e.activation(out=o, in_=i, func=func, bias=bias, scale=scale, accum_out=accum),
                    reads, writes)

    def tt(eng, o, a, b, op, reads, writes):
        return P.op(eng, lambda e: e.tensor_tensor(out=o, in0=a, in1=b, op=op), reads, writes)

    def ts(eng, o, a, s1, s2, op0, op1, reads, writes):
        if s2 is None:
            return P.op(eng, lambda e: e.tensor_scalar(out=o, in0=a, scalar1=s1, scalar2=None, op0=op0), reads, writes)
        return P.op(eng, lambda e: e.tensor_scalar(out=o, in0=a, scalar1=s1, scalar2=s2, op0=op0, op1=op1), reads, writes)

    def stt(eng, o, a, s, b, op0, op1, reads, writes):
        return P.op(eng, lambda e: e.scalar_tensor_tensor(out=o, in0=a, scalar=s, in1=b, op0=op0, op1=op1), reads, writes)

    def cp(eng, o, i, reads, writes):
        if eng == "act":
            return act(o, i, AF.Copy, reads, writes)
        return P.op(eng, lambda e: e.tensor_copy(out=o, in_=i), reads, writes)

    def ms(eng, o, v, writes):
        return P.op(eng, lambda e: e.memset(o, v), (), writes)

    def recip(o, i, reads, writes):
        return P.op("dve", lambda e: e.reciprocal(out=o, in_=i), reads, writes)

    def rstd_of(o, ss, n, reads, writes):
        act(o, ss, AF.Ln, reads, writes, scale=1.0 / n, bias=EPS)
        act(o, o, AF.Exp, writes, writes, scale=-0.5)

    def phase_W():
        with ExitStack() as st:
            CW = 2048
            stg = Ring([sbt(st, [128, CW], F32) for _ in range(3)])
            outb = Ring([sbt(st, [128, CW], BF16) for _ in range(3)])
            gt = sbt(st, [128, DEPTH, 32], F32)
            bg = Buf()
            for l in range(DEPTH):
                P.dma("sp", gt[:, l, 0:8], g_attn[l], writes=[bg])
                P.dma("sp", gt[:, l, 8:10], g_q[l], writes=[bg])
                P.dma("sp", gt[:, l, 10:11], g_kv[l], writes=[bg])
                P.dma("sp", gt[:, l, 11:19], g_mix[l], writes=[bg])
                P.dma("sp", gt[:, l, 19:27], g_mlp[l], writes=[bg])
            engs = ["dve", "act"]
            cnt = [0]

            def conv(src, dst, rows, cols, gain):
                for c0 in range(0, cols, CW):
                    cw = min(CW, cols - c0)
                    s_t, s_b = stg.next()
                    o_t, o_b = outb.next()
                    P.dma("sp", s_t[0:rows, 0:cw], src[:, c0:c0 + cw], writes=[s_b])
                    e = engs[cnt[0] % 2]
                    cnt[0] += 1
                    if gain is None:
                        cp(e, o_t[0:rows, 0:cw], s_t[0:rows, 0:cw], [s_b], [o_b])
                    elif e == "act":
                        act(o_t[0:rows, 0:cw], s_t[0:rows, 0:cw], AF.Copy, [s_b, bg], [o_b], scale=gain)
                    else:
                        ts(e, o_t[0:rows, 0:cw], s_t[0:rows, 0:cw], gain, None, ALU.mult, None, [s_b, bg], [o_b])
                    P.dma("pool", dst[:, c0:c0 + cw], o_t[0:rows, 0:cw], reads=[o_b])

            for k, shp in cshape.items():
                conv(cin[k], cb[k], shp[0], shp[1], None)
            for l in range(DEPTH):
                for k in range(8):
                    conv(w_in[l, k * 128:(k + 1) * 128, :], Win_b[l, k * 128:(k + 1) * 128, :], 128, WINC, gt[:, l, k:k + 1])
                for k in range(16):
                    conv(w1k[l, k * 128:(k + 1) * 128, :], W1k_b[l, k * 128:(k + 1) * 128, :], 128, 256, None)
                    conv(w1v[l, k * 128:(k + 1) * 128, :], W1v_b[l, k * 128:(k + 1) * 128, :], 128, 256, None)
                for k in range(2):
                    conv(w2k[l, k * 128:(k + 1) * 128, :], W2k_b[l, k * 128:(k + 1) * 128, :], 128, 64, None)
                    conv(w2v[l, k * 128:(k + 1) * 128, :], W2v_b[l, k * 128:(k + 1) * 128, :], 128, 64, None)
                    conv(wq[l, k * 128:(k + 1) * 128, :], Wq_b[l, k * 128:(k + 1) * 128, :], 128, 1024, gt[:, l, 8 + k:9 + k])
                conv(posk[l], Pk_b[l], 64, 32, None)
                conv(posv[l], Pv_b[l], 64, 32, None)
                conv(wkv[l], Wkv_b[l], 128, 1024, gt[:, l, 10:11])
                for k in range(8):
                    conv(w_out[l, k * 128:(k + 1) * 128, :], Wout_b[l, k * 128:(k + 1) * 128, :], 128, D, gt[:, l, 11 + k:12 + k])
                    conv(w_ff1[l, k * 128:(k + 1) * 128, :], W1_b[l, k * 128:(k + 1) * 128, :], 128, HID, gt[:, l, 19 + k:20 + k])
                for k in range(32):
                    conv(w_ff2[l, k * 128:(k + 1) * 128, :], W2_b[l, k * 128:(k + 1) * 128, :], 128, D, None)
            P.flush()

    def phase_A(l):
        xsrc = x_in if l == 0 else xs
        with ExitStack() as st:
            Wt = sbt(st, [128, 8, WINC], BF16)
            bW = Buf()
            for k in range(8):
                P.dma("sp", Wt[:, k, :], Win_b[l, k * 128:(k + 1) * 128, :], writes=[bW])
            ident = sbt(st, [128, 128], BF16)
            bI = Buf()
            P.dma("sp", ident[:], cb["identB"], writes=[bI])
            ones = sbt(st, [128, 128], BF16)
            bO = Buf()
            ms("pool", ones[:], 1.0, [bO])
            xr = Ring([sbt(st, [128, D], F32) for _ in range(3)])
            xnr = Ring([sbt(st, [128, D], BF16) for _ in range(5)])
            junk = sbt(st, [128, D], BF16)
            bJ = Buf()
            statr = Ring([sbt(st, [128, 4], F32) for _ in range(5)])
            hTr = Ring([sbt(st, [128, 8, 512], BF16) for _ in range(2)])
            rAr = Ring([sbt(st, [128, 2, 512], F32) for _ in range(2)])
            rBr = Ring([sbt(st, [128, 512], F32) for _ in range(2)])
            t1r = Ring([sbt(st, [128, 512], F32) for _ in range(3)])
            t2r = Ring([sbt(st, [128, 512], F32) for _ in range(3)])
            obr = Ring([sbt(st, [128, 512], BF16) for _ in range(4)])
            cqr = Ring([sbt(st, [128, 3, 512], F32) for _ in range(2)])
            sqr = Ring([sbt(st, [128, 3, 512], BF16) for _ in range(2)])
            rsr = Ring([sbt(st, [128, 512], F32) for _ in range(2)])
            tmr = Ring([sbt(st, [128, 280], BF16) for _ in range(3)])
            gtr = Ring([sbt(st, [128, 24], F32) for _ in range(3)])
            ptr = Ring([PS[0], PS[1]]); ptr.bufs = [PB[0], PB[1]]
            pfr = Ring([PS[2], PS[3], PS[4], PS[5]]); pfr.bufs = [PB[2], PB[3], PB[4], PB[5]]
            pmr = Ring([PS[6], PS[7]]); pmr.bufs = [PB[6], PB[7]]
            hts = {}

            def normT(tg):
                c0 = tg * 512
                hT, bH = hTr.next()
                rA, bRA = rAr.next()
                rB, bRB = rBr.next()
                P.dma("sp", rA[:, 0, :], ropeA[0, :, c0:c0 + 512], writes=[bRA])
                P.dma("sp", rA[:, 1, :], ropeA[1, :, c0:c0 + 512], writes=[bRA])
                P.dma("sp", rB[:], ropeB[:, c0:c0 + 512], writes=[bRB])
                xns = []
                for j in range(4):
                    t = tg * 4 + j
                    xt, bX = xr.next()
                    P.dma("sp", xt[:], xsrc[t * 128:(t + 1) * 128, :], writes=[bX])
                    sx, bS = statr.next()
                    act(junk[:], xt[:], AF.Square, [bX], [bJ, bS], accum=sx[:, 0:1])
                    rstd_of(sx[:, 1:2], sx[:, 0:1], D, [bS], [bS])
                    xn, bN = xnr.next()
                    ts("dve", xn[:], xt[:], sx[:, 1:2], None, ALU.mult, None, [bX, bS], [bN])
                    xns.append((xn, bN))
                hts[tg] = (hT, bH, rA, bRA, rB, bRB, xns)

            def normT2(tg):
                hT, bH, rA, bRA, rB, bRB, xns = hts[tg]
                for j in range(4):
                    xn, bN = xns[j]
                    pt, bP = ptr.next()
                    ptb = pt[:].bitcast(BF16)
                    for k in range(8):
                        tr(ptb[:, k * 128:(k + 1) * 128], xn[:, k * 128:(k + 1) * 128], ident[:], [bN, bI], [bP])
                    cp("act" if j % 2 else "dve", hT[:, :, j * 128:(j + 1) * 128],
                       ptb[:, 0:1024].rearrange("p (k t) -> p k t", k=8), [bP], [bH])

            def proj(tg):
                c0 = tg * 512
                hT, bH, rA, bRA, rB, bRB, _xns = hts.pop(tg)
                cq, bCQ = cqr.next()
                sq, bSQ = sqr.next()
                QaTf = QaT.rearrange("h d s -> (h d) s")
                KsTf = KsT.rearrange("g d s -> (g d) s")
                KwTf = KwT.rearrange("g d s -> (g d) s")
                for ct in range(19):
                    if ct < 14 and ct % 2 == 1:
                        continue
                    pf, bF = pfr.next()
                    for k in range(8):
                        mm(pf[:], Wt[:, k, ct * 128:(ct + 1) * 128], hT[:, k, :], k == 0, k == 7, [bW, bH], [bF])
                    if ct < 14:
                        pg, bG2 = pfr.next()
                        for k in range(8):
                            mm(pg[:], Wt[:, k, (ct + 1) * 128:(ct + 2) * 128], hT[:, k, :], k == 0, k == 7, [bW, bH], [bG2])
                        t1, b1 = t1r.next()
                        t2, b2 = t2r.next()
                        ob, bOB = obr.next()
                        tt("dve", t1[:, :], pg[:, :], rA[:, 1, :], ALU.mult, [bG2, bRA], [b1])
                        tt("dve", t2[:, :], pf[:, :], rA[:, 0, :], ALU.mult, [bF, bRA], [b2])
                        tt("pool", ob[:, :], t1[:, :], t2[:, :], ALU.add, [b1, b2], [bOB])
                        if ct < 8:
                            pr2 = ct // 2
                            dst = QaTf[pr2 * 128:(pr2 + 1) * 128, c0:c0 + 512]
                        else:
                            br = (ct - 8) // 2
                            dst = (KcT if br == 0 else (KsTf if br == 1 else KwTf))[:, c0:c0 + 512]
                        P.dma("sp", dst, ob[:, :], reads=[bOB])
                    elif ct == 14:
                        ob, bOB = obr.next()
                        cp("act", ob[:], pf[:], [bF], [bOB])
                        P.dma("sp", VcT[:, c0:c0 + 512], ob[:], reads=[bOB])
                    elif ct < 18:
                        i = ct - 15
                        act(sq[:, i, :], pf[:], AF.Square, [bF], [bSQ])
                        cp("dve", cq[:, i, :], pf[:], [bF], [bCQ])
                        if ct == 16 or ct == 17:
                            pf2, bF2 = pfr.next()
                            ks = (0, 1) if ct == 16 else (2,)
                            for n, kk in enumerate(ks):
                                mm(pf2[:], ones[:], sq[:, kk, :], n == 0, n == len(ks) - 1, [bO, bSQ], [bF2])
                            rs, bRS = rsr.next()
                            rstd_of(rs[:], pf2[:], 128.0 * len(ks), [bF2], [bRS])
                            for kk in ks:
                                ob, bOB = obr.next()
                                tt("dve", ob[:], cq[:, kk, :], rs[:], ALU.mult, [bCQ, bRS], [bOB])
                                dst = cqnT[kk * 128:(kk + 1) * 128, c0:c0 + 512] if kk < 2 else ckvnT[:, c0:c0 + 512]
                                P.dma("sp", dst, ob[:], reads=[bOB])
                    else:
                        t1, b1 = t1r.next()
                        t2, b2 = t2r.next()
                        ob, bOB = obr.next()
                        tt("dve", t1[0:32, :], pf[0:32, :], rB[0:32, :], ALU.mult, [bF, bRB], [b1])
                        tt("dve", t2[0:32, :], pf[32:64, :], rB[32:64, :], ALU.mult, [bF, bRB], [b2])
                        tt("pool", ob[0:32, :], t1[0:32, :], t2[0:32, :], ALU.add, [b1, b2], [bOB])
                        P.dma("sp", krT[:, c0:c0 + 512], ob[0:32, :], reads=[bOB])
                for j in range(4):
                    t = tg * 4 + j
                    pm, bM = pmr.next()
                    for k in range(8):
                        mm(pm[:, 0:280], hT[:, k, j * 128:(j + 1) * 128], Wt[:, k, TM0:TM0 + 280], k == 0, k == 7,
                           [bW, bH], [bM])
                    tm, bT = tmr.next()
                    cp("act", tm[:, 0:256], pm[:, 0:256], [bM], [bT])
                    P.dma("sp", Vs[t * 128:(t + 1) * 128, :], tm[:, 0:128], reads=[bT])
                    P.dma("sp", Vw[t * 128:(t + 1) * 128, :], tm[:, 128:256], reads=[bT])
                    gt_, bG = gtr.next()
                    act(gt_[:], pm[:, 256:280], AF.Exp, [bM], [bG], scale=-1.0)
                    ts("dve", gt_[:], gt_[:], 1.0, None, ALU.add, None, [bG], [bG])
                    recip(gt_[:], gt_[:], [bG], [bG])
                    P.dma("sp", gates[t * 128:(t + 1) * 128, :], gt_[:], reads=[bG])

            normT(0)
            normT2(0)
            for tg in range(NG):
                if tg + 1 < NG:
                    normT(tg + 1)
                proj(tg)
                if tg + 1 < NG:
                    normT2(tg + 1)
            P.flush()

    def phase_B(l):
        with ExitStack() as st:
            srcs = []
            for (XT, W1b, W2b, Pb) in ((KcT, W1k_b, W2k_b, Pk_b), (VcT, W1v_b, W2v_b, Pv_b)):
                xt = sbt(st, [128, S], BF16); bx = Buf()
                P.dma("sp", xt[:], XT, writes=[bx])
                w1 = sbt(st, [128, 32, 256], BF16); b1 = Buf()
                src = W1b[l].rearrange("(l d) h -> d l h", d=64)
                P.dma("sp", w1[0:64], src, writes=[b1])
                P.dma("sp", w1[64:128], src, writes=[b1])
                w2 = sbt(st, [128, 2, 64], BF16); b2 = Buf()
                P.dma("sp", w2[:], W2b[l].rearrange("(c p) d -> p c d", p=128), writes=[b2])
                pp = sbt(st, [64, 32], BF16); bp = Buf()
                P.dma("sp", pp[:], Pb[l], writes=[bp])
                srcs.append((xt, bx, w1, b1, w2, b2, pp, bp))
            bias = sbt(st, [128, 8], F32); bB = Buf()
            hid = Ring([sbt(st, [128, 2, 256], BF16) for _ in range(2)])
            ob = Ring([sbt(st, [128, 256], BF16) for _ in range(3)])
            pr = Ring(PS[0:4]); pr.bufs = PB[0:4]
            for kv, (xt, bx, w1, b1, w2, b2, pp, bp) in enumerate(srcs):
                for c in range(2):
                    pb_, bPb = pr.next()
                    for li in range(32):
                        mm(pb_[:, 0:1], w1[0:64, li, c * 128:(c + 1) * 128], pp[:, li:li + 1], li == 0, li == 31,
                           [b1, bp], [bPb])
                    cp("dve", bias[:, kv * 2 + c:kv * 2 + c + 1], pb_[:, 0:1], [bPb], [bB])
                for g in range(2):
                    hd, bh = hid.next()
                    for c in range(2):
                        ph, bPh = pr.next()
                        for li in range(32):
                            mm(ph[:, 0:NCMP], w1[g * 64:(g + 1) * 64, li, c * 128:(c + 1) * 128],
                               xt[g * 64:(g + 1) * 64, li:li + 16 * (NCMP - 1) + 1:16], li == 0, li == 31,
                               [b1, bx], [bPh])
                        act(hd[:, c, 0:NCMP], ph[:, 0:NCMP], AF.Silu, [bPh, bB], [bh],
                            bias=bias[:, kv * 2 + c:kv * 2 + c + 1])
                    if kv == 0:
                        po, bPo = pr.next()
                        for c in range(2):
                            mm(po[0:64, 0:NCMP], w2[:, c, :], hd[:, c, 0:NCMP], c == 0, c == 1, [b2, bh], [bPo])
                        o_, bo = ob.next()
                        cp("dve", o_[0:64, 0:NCMP], po[0:64, 0:NCMP], [bPo], [bo])
                        P.dma("pool", KcmpT[g, :, 0:NCMP], o_[0:64, 0:NCMP], reads=[bo])
                    else:
                        for n0 in range(0, NCMP, 128):
                            cnt = min(128, NCMP - n0)
                            po, bPo = pr.next()
                            for c in range(2):
                                mm(po[0:cnt, 0:64], hd[:, c, n0:n0 + cnt], w2[:, c, :], c == 0, c == 1, [b2, bh], [bPo])
                            o_, bo = ob.next()
                            cp("dve", o_[0:cnt, 0:64], po[0:cnt, 0:64], [bPo], [bo])
                            P.dma("pool", Vcmp[n0:n0 + cnt, g, :], o_[0:cnt, 0:64], reads=[bo])
            P.flush()

    class AttnCtx:
        pass

    def attn_setup(st):
        a = AttnCtx()
        a.sr = Ring(PS[0:3]); a.sr.bufs = PB[0:3]
        a.orr = Ring(PS[3:5]); a.orr.bufs = PB[3:5]
        a.tr = Ring(PS[5:7]); a.tr.bufs = PB[5:7]
        a.pr = Ring([sbt(st, [128, 512], BF16) for _ in range(4)])
        a.otr = Ring([sbt(st, [65, 512], F32) for _ in range(3)])
        a.ident = sbt(st, [128, 128], BF16); a.bI = Buf()
        P.dma("sp", a.ident[:], cb["identB"], writes=[a.bI])
        a.identf = sbt(st, [128, 128], F32); a.bIf = Buf()
        P.dma("sp", a.identf[:], cin["identB"], writes=[a.bIf])
        a.triC = sbt(st, [128, 2048], BF16); a.bTC = Buf()
        P.dma("sp", a.triC[:], cb["triC"], writes=[a.bTC])
        a.triW = sbt(st, [128, 2048], BF16); a.bTW = Buf()
        P.dma("sp", a.triW[:], cb["triW"], writes=[a.bTW])
        a.smr = Ring([sbt(st, [128, 16], F32) for _ in range(6)])
        return a

    class Pipe:
        def __init__(self, a, L=2, PRE=6, DEFER=3):
            self.a = a
            self.L = L
            self.PRE = PRE
            self.DEFER = DEFER
            self.runs = []

        def add(self, tiles, scale, q_ap, q_bufs, facs_fn, acc_fn, pre=None, post=None):
            self.runs.append([tiles, scale, q_ap, q_bufs, facs_fn, acc_fn, pre, post])

        def emit(self):
            a = self.a
            runs = self.runs
            self.runs = []
            jobs = []
            firsts = []
            for r, run in enumerate(runs):
                firsts.append(len(jobs))
                for i in range(len(run[0])):
                    jobs.append((r, i))
            nj = len(jobs)
            pre_done = 0
            sc_state = {}
            acc_state = {}
            pending = []
            for idx in range(nj + self.L):
                if idx < nj:
                    while pre_done < len(runs) and firsts[pre_done] - self.PRE <= idx:
                        if runs[pre_done][6] is not None:
                            runs[pre_done][6]()
                        pre_done += 1
                    r, i = jobs[idx]
                    tiles, scale, q_ap, q_bufs = runs[r][0:4]
                    kT, kb, va, vb, m = tiles[i]
                    cnt = kT.shape[1]
                    s_, bS = a.sr.next()
                    mm(s_[0:cnt, :], kT, q_ap(0, 512), True, m is None, list(kb) + list(q_bufs), [bS])
                    if m is not None:
                        mm(s_[0:cnt, :], m[0], m[1], False, True, list(m[2]), [bS])
                    sc_state[idx] = (s_, bS, cnt)
                k = idx - self.L
                if k >= 0:
                    r, i = jobs[k]
                    tiles, scale = runs[r][0:2]
                    kT, kb, va, vb, m = tiles[i]
                    s_, bS, cnt = sc_state.pop(k)
                    if i == 0:
                        acc_state[r] = a.orr.next()
                    po, bPo = acc_state[r]
                    p_, bP = a.pr.next()
                    act(p_[0:cnt, :], s_[0:cnt, :], AF.Exp, [bS], [bP], scale=scale)
                    mm(po[0:65, :], va, p_[0:cnt, :], i == 0, i == len(tiles) - 1, list(vb) + [bP], [bPo])
                    if i == len(tiles) - 1:
                        del acc_state[r]
                        ot, bOT = a.otr.next()
                        cp("dve", ot[:, :], po[0:65, :], [bPo], [bOT])
                        pending.append([self.DEFER, (lambda ot=ot, bOT=bOT, r=r: attn_finish2(a, ot, bOT, runs[r][4], runs[r][5], runs[r][7]))])
                for pnd in pending:
                    pnd[0] -= 1
                while pending and pending[0][0] <= 0:
                    pending.pop(0)[1]()
            for pnd in pending:
                pnd[1]()

    def attn_finish2(a, ot, bOT, facs_fn, acc_fn, post):
        ptk, bPT = a.tr.next()
        for j in range(4):
            tr(ptk[:, j * 65:(j + 1) * 65], ot[0:65, j * 128:(j + 1) * 128], a.identf[0:65, 0:65], [bOT, a.bIf], [bPT])
        sm, bSM = a.smr.next()
        v = ptk[:, 0:260].rearrange("p (j c) -> p j c", c=65)
        ts("dve", sm[:, 0:4], v[:, :, 64], 1e-30, None, ALU.max, None, [bPT], [bSM])
        recip(sm[:, 4:8], sm[:, 0:4], [bSM], [bSM])
        fac = facs_fn(sm, bSM)
        for j in range(4):
            acc_fn(j, ptk[:, j * 65:j * 65 + 64], fac[:, j:j + 1], [bPT, bSM])
        if post is not None:
            post()

    def phase_C(l):
        sc = DK ** -0.5
        with ExitStack() as st:
            a = attn_setup(st)
            KsA = []
            KwS = []
            for g in range(G):
                t_ = sbt(st, [128, S], BF16); b_ = Buf()
                P.dma("sp", t_[0:64, :], KsT[g], writes=[b_])
                P.dma("sp", t_[64:128, :], cb["eind"], writes=[b_])
                KsA.append((t_, b_))
                t2_ = sbt(st, [128, S], BF16); b2_ = Buf()
                ms("pool", t2_[64:128, :], 0.0, [b2_])
                P.dma("sp", t2_[0:64, :], KwT[g], writes=[b2_])
                KwS.append((t2_, b2_))
            VsA = sbt(st, [128, NT, G, 65], BF16); bVs = Buf()
            VwA = sbt(st, [128, NT, G, 65], BF16); bVw = Buf()
            ms("pool", VsA[:], 1.0, [bVs])
            ms("pool", VwA[:], 1.0, [bVw])
            for g in range(G):
                P.dma("sp", VsA[:, :, g, 0:64], Vs[:, g * 64:(g + 1) * 64].rearrange("(t p) d -> p t d", p=128), writes=[bVs])
                P.dma("sp", VwA[:, :, g, 0:64], Vw[:, g * 64:(g + 1) * 64].rearrange("(t p) d -> p t d", p=128), writes=[bVw])
            KcS = sbt(st, [64, G, 256], BF16); bKc = Buf()
            P.dma("sp", KcS[:, :, 0:NCMP], KcmpT[:, :, 0:NCMP].rearrange("g d n -> d g n"), writes=[bKc])
            VcA = sbt(st, [128, 2, G, 65], BF16); bVc = Buf()
            ms("pool", VcA[:], 1.0, [bVc])
            for n0 in range(0, NCMP, 128):
                cnt = min(128, NCMP - n0)
                P.dma("sp", VcA[0:cnt, n0 // 128, :, 0:64], Vcmp[n0:n0 + cnt], writes=[bVc])
            gat = sbt(st, [128, NT, 24], F32); bGa = Buf()
            P.dma("sp", gat[:], gates.rearrange("(t p) c -> p t c", p=128), writes=[bGa])
            cmT = sbt(st, [33, 512], BF16); bCm = Buf()
            P.dma("sp", cmT[:], cb["cmT"], writes=[bCm])
            jb = sbt(st, [33, 400], BF16); bJb = Buf()
            P.dma("sp", jb[:], cb["jbig"], writes=[bJb])
            sv = sbt(st, [128, 128], F32); sa = sbt(st, [128, 128], F32); bSel = Buf()
            P.dma("sp", sv[:], selv, writes=[bSel])
            P.dma("sp", sa[:], sela, writes=[bSel])
            Qr = Ring([sbt(st, [128, NH, 512], BF16) for _ in range(2)])
            Qnb = [Buf(), Buf()]
            Er = Ring([sbt(st, [128, 256], F32) for _ in range(33)])
            smP = Ring([sbt(st, [128, 4], F32) for _ in range(33)])
            impr = Ring([sbt(st, [128, 264], F32) for _ in range(9)])
            wkr = Ring([sbt(st, [128, 320], F32) for _ in range(9)])
            accr = Ring([sbt(st, [128, 4, 512], F32) for _ in range(2)])
            fcr = Ring([sbt(st, [128, 8], F32) for _ in range(3)])
            mxr = Ring([sbt(st, [128, 4, 512], BF16) for _ in range(2)])
            junk = sbt(st, [128, 512], BF16); bJ = Buf()
            scr = Ring(PS[7:8]); scr.bufs = PB[7:8]
            pnb, bPnb = PS[7], PB[7]
            NBs = []
            for _ in range(8):
                t_ = sbt(st, [128, 128], BF16); b_ = Buf()
                ms("pool", t_[:], 0.0, [b_])
                NBs.append((t_, b_))
            Qs = {}
            wks = {}

            def load_q(qt):
                Q, bQ = Qr.next()
                bQn = Qnb[(Qr.i - 1) % 2]
                P.dma("sp", Q[0:64, :, :], QaT[:, :, qt * 512:(qt + 1) * 512].rearrange("h d s -> d h s"), writes=[bQ])
                Qs[qt] = (Q, bQ, bQn)

            def prelude1(qt, j, g):
                Q, bQ, bQn = Qs[qt]
                qi = qt * 4 + j
                ncnt = min(NCMP, 8 * qi + 7)
                imp, bImp = impr.next()
                ms("pool", imp[:], 0.0, [bImp])
                for r in range(4):
                    h = 4 * g + r
                    s_, bS = scr.next()
                    mm(s_[:, 0:ncnt], Q[0:64, h, j * 128:(j + 1) * 128], KcS[:, g, 0:ncnt], True, False, [bQ, bKc], [bS])
                    a0 = 258 - 8 * qi
                    mm(s_[:, 0:ncnt], cmT[0:9, 0:128], jb[0:9, a0:a0 + ncnt], False, True, [bCm, bJb], [bS])
                    E, bE = Er.next()
                    sm, bSM = smP.next()
                    act(E[:, 0:ncnt], s_[:, 0:ncnt], AF.Exp, [bS], [bE, bSM], scale=sc, accum=sm[:, 0:1])
                    ts("dve", sm[:, 1:2], sm[:, 0:1], 1e-30, None, ALU.max, None, [bSM], [bSM])
                    recip(sm[:, 2:3], sm[:, 1:2], [bSM], [bSM])
                    ts("pool", E[:, 0:ncnt], E[:, 0:ncnt], sm[:, 2:3], None, ALU.mult, None, [bE, bSM], [bE])
                    tt("pool", imp[:, 1:1 + ncnt], imp[:, 1:1 + ncnt], E[:, 0:ncnt], ALU.add, [bE, bImp], [bImp])
                wk, bWk = wkr.next()
                ib = wk[:, 0:64]
                nv = 4 * (NBLK - 1) + 1
                tt("pool", ib[:, 0:NBLK], imp[:, 1:1 + nv:4], imp[:, 2:2 + nv:4], ALU.add, [bImp], [bWk])
                tt("pool", ib[:, 0:NBLK], ib[:, 0:NBLK], imp[:, 3:3 + nv:4], ALU.add, [bImp, bWk], [bWk])
                ts("pool", ib[:, 0:NBLK], ib[:, 0:NBLK], 2.0, None, ALU.mult, None, [bWk], [bWk])
                tt("pool", ib[:, 0:NBLK], ib[:, 0:NBLK], imp[:, 0:nv:4], ALU.add, [bImp, bWk], [bWk])
                tt("pool", ib[:, 0:NBLK], ib[:, 0:NBLK], imp[:, 4:4 + nv:4], ALU.add, [bImp, bWk], [bWk])
                s0 = 62 - 2 * qi
                if NBLK < 64:
                    ms("pool", wk[:, NBLK:64], -1.0, [bWk])
                tt("pool", ib[:, 0:NBLK], ib[:, 0:NBLK], sv[:, s0:s0 + NBLK], ALU.mult, [bWk, bSel], [bWk])
                tt("pool", ib[:, 0:NBLK], ib[:, 0:NBLK], sa[:, s0:s0 + NBLK], ALU.add, [bWk, bSel], [bWk])
                ms("pool", wk[:, 0:1], 20000.0, [bWk])
                wks[(qt, j, g)] = (wk, bWk)

            def prelude1b(qt, j, g):
                wk, bWk = wks.pop((qt, j, g))
                P.op("dve", lambda e, o=wk[:, 64:72], i=wk[:, 0:64]: e.max(out=o, in_=i), [bWk], [bWk])
                P.op("dve", lambda e, o=wk[:, 128:192], r_=wk[:, 64:72], i=wk[:, 0:64]:
                     e.match_replace(out=o, in_to_replace=r_, in_values=i, imm_value=-1e30), [bWk], [bWk])
                P.op("dve", lambda e, o=wk[:, 72:80], i=wk[:, 128:192]: e.max(out=o, in_=i), [bWk], [bWk])
                ts("dve", wk[:, 192:256], wk[:, 0:64], wk[:, 79:80], -NEG, ALU.is_ge, ALU.mult, [bWk], [bWk])
                NBt, bNB = NBs[j * 2 + g]
                ts("dve", NBt[:, 64:128], wk[:, 192:256], NEG, None, ALU.add, None, [bWk], [bNB])

            def prelude2(qt, j, g):
                Q, bQ, bQn = Qs[qt]
                NBt, bNB = NBs[j * 2 + g]
                pnbb = pnb[:].bitcast(BF16)
                tr(pnbb[:, 0:128], NBt[:], a.ident[:], [bNB, a.bI], [bPnb])
                for r in range(4):
                    cp("act" if r % 2 else "dve", Q[64:128, 4 * g + r, j * 128:(j + 1) * 128],
                       pnbb[64:128, 0:128], [bPnb], [bQn])

            load_q(0)
            for j in range(4):
                for g in range(G):
                    prelude1(0, j, g)
            for j in range(4):
                for g in range(G):
                    prelude1b(0, j, g)
                    prelude2(0, j, g)
            for qt in range(NG):
                q0 = qt * 512
                Q, bQ, bQn = Qs[qt]
                nxt = qt + 1 < NG
                if nxt:
                    load_q(qt + 1)
                    for j in range(4):
                        for g in range(G):
                            prelude1(qt + 1, j, g)
                acc, bAcc = accr.next()
                pipe = Pipe(a)

                def add_run(h, bi, br, tiles, qfun, qb, pre):
                    fc, bFc = fcr.next()

                    def facs(sm, bSM, fc=fc, bFc=bFc, h=h, br=br, qt=qt):
                        tt("dve", fc[:, 0:4], sm[:, 4:8], gat[:, qt * 4:qt * 4 + 4, h * 3 + br], ALU.mult,
                           [bSM, bGa], [bFc])
                        return fc

                    def accf(j, otok, f, reads, acc=acc, bAcc=bAcc, h=h, bi=bi, bFc=bFc):
                        dst = acc[:, j, h * 64:(h + 1) * 64]
                        if bi == 0:
                            ts("dve", dst, otok, f, None, ALU.mult, None, reads + [bFc], [bAcc])
                        else:
                            stt("dve", dst, otok, f, dst, ALU.mult, ALU.add, reads + [bFc, bAcc], [bAcc])
                    pipe.add(tiles, sc, qfun, qb, facs, accf, pre=pre)

                for h in range(NH):
                    g = h // 4
                    qf = lambda lo, hi, Q=Q, h=h: Q[0:64, h, lo:hi]
                    ncq = min(NCMP, 32 * qt + 31)
                    tiles = []
                    for n0 in range(0, ncq, 128):
                        cnt = min(128, ncq - n0)
                        a0 = n0 - 32 * qt + 258
                        tiles.append((KcS[:, g, n0:n0 + cnt], [bKc], VcA[0:cnt, n0 // 128, g, :], [bVc],
                                      (jb[:, a0:a0 + cnt], cmT[:, :], [bJb, bCm])))
                    add_run(h, 0, 0, tiles, qf, [bQ], None)
                    tiles = []
                    for jp in range(4):
                        k0 = q0 - 512 + 128 * jp
                        if k0 < 0:
                            continue
                        tiles.append((KwS[g][0][:, k0:k0 + 128], [KwS[g][1]], VwA[:, k0 // 128, g, :], [bVw],
                                      (a.ident[:], a.triW[:, jp * 512:(jp + 1) * 512], [a.bI, a.bTW])))
                    for jn in range(4):
                        k0 = q0 + 128 * jn
                        tiles.append((KwS[g][0][:, k0:k0 + 128], [KwS[g][1]], VwA[:, k0 // 128, g, :], [bVw],
                                      (a.ident[:], a.triC[:, jn * 512:(jn + 1) * 512], [a.bI, a.bTC])))
                    add_run(h, 1, 2, tiles, (lambda lo, hi, Q=Q, h=h: Q[:, h, lo:hi]), [bQ, bQn], None)
                for h in range(NH):
                    g = h // 4
                    qfa = lambda lo, hi, Q=Q, h=h: Q[:, h, lo:hi]
                    tiles = []
                    for kt in range(4 * qt + 4):
                        k0 = kt * 128
                        jn = kt - 4 * qt
                        if jn < 0:
                            tiles.append((KsA[g][0][:, k0:k0 + 128], [KsA[g][1]], VsA[:, kt, g, :], [bVs], None))
                        else:
                            tiles.append((KsA[g][0][:, k0:k0 + 128], [KsA[g][1]], VsA[:, kt, g, :], [bVs],
                                          (a.ident[:], a.triC[:, jn * 512:(jn + 1) * 512], [a.bI, a.bTC])))
                    def pre2(qt=qt, h=h):
                        prelude1b(qt + 1, h // 2, h % 2)
                    add_run(h, 2, 1, tiles, qfa, [bQ, bQn], pre2 if nxt else None)
                pipe.emit()
                if nxt:
                    for j in range(4):
                        for g in range(G):
                            prelude2(qt + 1, j, g)
                mx, bMx = mxr.next()
                for j in range(4):
                    sm, bSM = a.smr.next()
                    act(junk[:], acc[:, j, :], AF.Square, [bAcc], [bJ, bSM], accum=sm[:, 0:1])
                    rstd_of(sm[:, 1:2], sm[:, 0:1], 512.0, [bSM], [bSM])
                    ts("dve", mx[:, j, :], acc[:, j, :], sm[:, 1:2], None, ALU.mult, None, [bAcc, bSM], [bMx])
                P.dma("pool", mixed[q0:q0 + 512, 0:512].rearrange("(j p) c -> p j c", p=128), mx[:], reads=[bMx])
            P.flush()

    def phase_D(l):
        sc = 96.0 ** -0.5
        with ExitStack() as st:
            a = attn_setup(st)
            ckv = sbt(st, [128, S], BF16); bCkv = Buf()
            P.dma("sp", ckv[:], ckvnT, writes=[bCkv])
            cqn = sbt(st, [128, 2, S], BF16); bCq = Buf()
            P.dma("sp", cqn[:], cqnT.rearrange("(c p) s -> p c s", p=128), writes=[bCq])
            wqt = sbt(st, [128, 2, 1024], BF16); bWq = Buf()
            P.dma("sp", wqt[:], Wq_b[l].rearrange("(c p) n -> p c n", p=128), writes=[bWq])
            wkt = sbt(st, [128, 1024], BF16); bWk = Buf()
            P.dma("sp", wkt[:], Wkv_b[l], writes=[bWk])
            VA = sbt(st, [128, NT, NH, 65], BF16); bVA = Buf()
            ms("pool", VA[:], 1.0, [bVA])
            rBr = Ring([sbt(st, [128, 512], F32) for _ in range(2)])
            bObD = Buf()
            pvr = Ring(PS[5:7]); pvr.bufs = PB[5:7]
            for t in range(NT):
                pv, bPv = pvr.next()
                mm(pv[:], ckv[:, t * 128:(t + 1) * 128], wkt[:, 512:1024], True, True, [bCkv, bWk], [bPv])
                cp("act" if t % 2 else "dve", VA[:, t, :, 0:64], pv[:].rearrange("p (h d) -> p h d", h=NH), [bPv], [bVA])
            Kr = Ring([sbt(st, [128, S], BF16) for _ in range(2)])
            Qr = Ring([sbt(st, [128, 512], BF16) for _ in range(4)])
            t1r = Ring([sbt(st, [128, 512], F32) for _ in range(2)])
            t2r = Ring([sbt(st, [128, 512], F32) for _ in range(2)])
            accr = Ring([sbt(st, [128, 4, 512], F32) for _ in range(2)])
            osr = Ring([sbt(st, [128, 4, 64], F32) for _ in range(5)])
            junk = sbt(st, [128, 512], BF16); bJ = Buf()
            mxr = Ring([sbt(st, [128, 4, 512], BF16) for _ in range(2)])
            pkr = Ring(PS[5:8]); pkr.bufs = PB[5:8]
            Ks = {}

            def build_k(h):
                K, bK = Kr.next()
                P.dma("sp", K[64:96, :], krT, writes=[bK])
                for tg in range(NG):
                    pk, bPk = pkr.next()
                    mm(pk[0:64, :], wkt[:, h * 64:(h + 1) * 64], ckv[:, tg * 512:(tg + 1) * 512], True, True, [bWk, bCkv], [bPk])
                    cp("dve", K[0:64, tg * 512:(tg + 1) * 512], pk[0:64, :], [bPk], [bK])
                Ks[h] = (K, bK)

            def build_q(h, qt, Q, bQ):
                q0 = qt * 512
                rBt, bRB = rBr.next()
                P.dma("sp", rBt[:], ropeB[:, q0:q0 + 512], writes=[bRB])
                pq, bPq = pkr.next()
                for c in range(2):
                    mm(pq[0:96, :], wqt[:, c, h * 128:h * 128 + 96], cqn[:, c, q0:q0 + 512], c == 0, c == 1, [bWq, bCq], [bPq])
                pr_, bPr = pkr.next()
                for c in range(2):
                    mm(pr_[0:32, :], wqt[:, c, h * 128 + 96:h * 128 + 128], cqn[:, c, q0:q0 + 512], c == 0, c == 1,
                       [bWq, bCq], [bPr])
                cp("dve", Q[0:64, :], pq[0:64, :], [bPq], [bQ])
                t1, b1 = t1r.next()
                t2, b2 = t2r.next()
                tt("dve", t1[64:96, :], pr_[0:32, :], rBt[0:32, :], ALU.mult, [bPr, bRB], [b1])
                tt("dve", t2[64:96, :], pq[64:96, :], rBt[64:96, :], ALU.mult, [bPq, bRB], [b2])
                tt("pool", Q[64:96, :], t1[64:96, :], t2[64:96, :], ALU.add, [b1, b2], [bQ])

            build_k(0)
            for h in range(NH):
                K, bK = Ks[h]
                pipe = Pipe(a, PRE=10)
                for qt in range(NG):
                    q0 = qt * 512
                    Q, bQ = Qr.next()
                    tiles = []
                    for kt in range(4 * qt + 4):
                        k0 = kt * 128
                        jn = kt - 4 * qt
                        if jn < 0:
                            tiles.append((K[0:96, k0:k0 + 128], [bK], VA[:, kt, h, :], [bVA], None))
                        else:
                            tiles.append((K[0:96, k0:k0 + 128], [bK], VA[:, kt, h, :], [bVA],
                                          (a.ident[:], a.triC[:, jn * 512:(jn + 1) * 512], [a.bI, a.bTC])))
                    os_, bOs = osr.next()

                    def facs(sm, bSM):
                        return sm[:, 4:8]

                    def accf(j, otok, f, reads, os_=os_, bOs=bOs):
                        ts("dve", os_[:, j, :], otok, f, None, ALU.mult, None, reads, [bOs])

                    def post(os_=os_, bOs=bOs, q0=q0, h=h):
                        P.dma("pool", obD[q0:q0 + 512, h * 64:(h + 1) * 64].rearrange("(j p) c -> p j c", p=128), os_[:],
                              reads=[bOs], writes=[bObD])

                    def pre(h=h, qt=qt, Q=Q, bQ=bQ):
                        build_q(h, qt, Q, bQ)
                        if qt == NG - 1 and h + 1 < NH:
                            build_k(h + 1)
                    pipe.add(tiles, sc, (lambda lo, hi, Q=Q: Q[0:96, lo:hi]), [bQ], facs, accf, pre=pre, post=post)
                pipe.emit()
            for qt in range(NG):
                mx, bMx = mxr.next()
                acc, bAcc = accr.next()
                P.dma("sp", acc[:], obD[qt * 512:(qt + 1) * 512, :].rearrange("(j p) c -> p j c", p=128),
                      reads=[bObD], writes=[bAcc])
                for j in range(4):
                    sm, bSM = a.smr.next()
                    act(junk[:], acc[:, j, :], AF.Square, [bAcc], [bJ, bSM], accum=sm[:, 0:1])
                    rstd_of(sm[:, 1:2], sm[:, 0:1], 512.0, [bSM], [bSM])
                    ts("dve", mx[:, j, :], acc[:, j, :], sm[:, 1:2], None, ALU.mult, None, [bAcc, bSM], [bMx])
                P.dma("pool", mixed[qt * 512:(qt + 1) * 512, 512:1024].rearrange("(j p) c -> p j c", p=128), mx[:],
                      reads=[bMx])
            P.flush()

    def phase_E1(l):
        xsrc = x_in if l == 0 else xs
        with ExitStack() as st:
            wo = sbt(st, [128, 8, D], BF16); bWo = Buf()
            P.dma("sp", wo[:], Wout_b[l].rearrange("(k p) n -> p k n", p=128), writes=[bWo])
            ident = sbt(st, [128, 128], BF16); bI = Buf()
            P.dma("sp", ident[:], cb["identB"], writes=[bI])
            mr = Ring([sbt(st, [128, D], BF16) for _ in range(3)])
            mTr = Ring([sbt(st, [128, 8, 128], BF16) for _ in range(3)])
            xr = Ring([sbt(st, [128, D], F32) for _ in range(4)])
            xnr = Ring([sbt(st, [128, D], F32) for _ in range(3)])
            hr = Ring([sbt(st, [128, D], BF16) for _ in range(4)])
            hTr = Ring([sbt(st, [128, 8, 128], BF16) for _ in range(3)])
            statr = Ring([sbt(st, [128, 4], F32) for _ in range(3)])
            junk = sbt(st, [128, D], BF16); bJ = Buf()
            ptr = Ring(PS[0:2]); ptr.bufs = PB[0:2]
            por = Ring([(PS[2], PS[3]), (PS[4], PS[5])]); por.bufs = [(PB[2], PB[3]), (PB[4], PB[5])]
            pt2 = Ring(PS[6:8]); pt2.bufs = PB[6:8]
            st1 = {}
            st2 = {}

            def s1(t):
                m_, bM = mr.next()
                P.dma("sp", m_[:], mixed[t * 128:(t + 1) * 128, :], writes=[bM])
                xt, bX = xr.next()
                P.dma("sp", xt[:], xsrc[t * 128:(t + 1) * 128, :], writes=[bX])
                pt, bP = ptr.next()
                ptb = pt[:].bitcast(BF16)
                for k in range(8):
                    tr(ptb[:, k * 128:(k + 1) * 128], m_[:, k * 128:(k + 1) * 128], ident[:], [bM, bI], [bP])
                mT, bMT = mTr.next()
                cp("act", mT[:], ptb[:, 0:1024].rearrange("p (k t) -> p k t", k=8), [bP], [bMT])
                st1[t] = (xt, bX, mT, bMT)

            def s2(t):
                xt, bX, mT, bMT = st1.pop(t)
                (p0, p1), (b0, b1) = por.next()
                xn, bXN = xnr.next()
                for half, (pp, bb) in enumerate(((p0, b0), (p1, b1))):
                    for k in range(8):
                        mm(pp[:], mT[:, k, :], wo[:, k, half * 512:(half + 1) * 512], k == 0, k == 7, [bMT, bWo], [bb])
                    tt("dve", xn[:, half * 512:(half + 1) * 512], pp[:], xt[:, half * 512:(half + 1) * 512], ALU.add,
                       [bb, bX], [bXN])
                P.dma("pool", xs[t * 128:(t + 1) * 128, :], xn[:], reads=[bXN])
                sx, bS = statr.next()
                act(junk[:], xn[:], AF.Square, [bXN], [bJ, bS], accum=sx[:, 0:1])
                rstd_of(sx[:, 1:2], sx[:, 0:1], D, [bS], [bS])
                hh, bHH = hr.next()
                ts("dve", hh[:], xn[:], sx[:, 1:2], None, ALU.mult, None, [bXN, bS], [bHH])
                st2[t] = (hh, bHH)

            def s3(t):
                hh, bHH = st2.pop(t)
                pq, bQ = pt2.next()
                pqb = pq[:].bitcast(BF16)
                for k in range(8):
                    tr(pqb[:, k * 128:(k + 1) * 128], hh[:, k * 128:(k + 1) * 128], ident[:], [bHH, bI], [bQ])
                hT, bHT = hTr.next()
                cp("act", hT[:], pqb[:, 0:1024].rearrange("p (k t) -> p k t", k=8), [bQ], [bHT])
                P.dma("pool", h2T[:, t * 128:(t + 1) * 128].rearrange("(k p) t -> p k t", p=128), hT[:], reads=[bHT])

            for t in range(NT + 3):
                if t < NT:
                    s1(t)
                if 0 <= t - 1 < NT:
                    s2(t - 1)
                if 0 <= t - 3 < NT:
                    s3(t - 3)
            P.flush()

    def phase_E2(l, last):
        with ExitStack() as st:
            w2 = sbt(st, [128, 32, D], BF16); bW2 = Buf()
            for c4 in range(4):
                P.dma("sp", w2[:, c4 * 8:(c4 + 1) * 8, :],
                      W2_b[l, c4 * 1024:(c4 + 1) * 1024, :].rearrange("(c p) n -> p c n", p=128), writes=[bW2])
            w1r = Ring([sbt(st, [128, 8, 512], BF16) for _ in range(2)])
            hTr = Ring([sbt(st, [128, 8, 512], BF16) for _ in range(2)])
            hdr = Ring([sbt(st, [128, 32, 512], BF16) for _ in range(1)])
            rr = Ring([sbt(st, [128, 512], F32) for _ in range(3)])
            xr = Ring([sbt(st, [128, D], F32) for _ in range(2)])
            xnr = Ring([sbt(st, [128, D], F32) for _ in range(2)])
            statr = Ring([sbt(st, [128, 4], F32) for _ in range(3)])
            junk = sbt(st, [128, D], BF16); bJ = Buf()
            gf = sbt(st, [128, D], F32); bGf = Buf()
            if last:
                P.dma("sp", gf[:], g_fin, writes=[bGf])
            p1r = Ring(PS[0:4]); p1r.bufs = PB[0:4]
            por = Ring([(PS[4], PS[5]), (PS[6], PS[7])]); por.bufs = [(PB[4], PB[5]), (PB[6], PB[7])]
            for tg in range(NG):
                c0 = tg * 512
                hT, bH = hTr.next()
                P.dma("sp", hT[:], h2T[:, c0:c0 + 512].rearrange("(k p) t -> p k t", p=128), writes=[bH])
                hd, bHd = hdr.next()
                for c4 in range(8):
                    w1, bW1 = w1r.next()
                    P.dma("sp", w1[:], W1_b[l, :, c4 * 512:(c4 + 1) * 512].rearrange("(k p) n -> p k n", p=128), writes=[bW1])
                    for cc in range(4):
                        c = c4 * 4 + cc
                        p1, bP1 = p1r.next()
                        for k in range(8):
                            mm(p1[:], w1[:, k, cc * 128:(cc + 1) * 128], hT[:, k, :], k == 0, k == 7, [bW1, bH], [bP1])
                        r_, bR = rr.next()
                        act(r_[:], p1[:], AF.Relu, [bP1], [bR])
                        tt("pool", hd[:, c, :], r_[:], r_[:], ALU.mult, [bR], [bHd])
                for j in range(4):
                    t = tg * 4 + j
                    xt, bX = xr.next()
                    P.dma("sp", xt[:], xs[t * 128:(t + 1) * 128, :], writes=[bX])
                    (p0, p1_), (b0, b1) = por.next()
                    xn, bXN = xnr.next()
                    for half, (pp, bb) in enumerate(((p0, b0), (p1_, b1))):
                        for c in range(32):
                            mm(pp[:], hd[:, c, j * 128:(j + 1) * 128], w2[:, c, half * 512:(half + 1) * 512],
                               c == 0, c == 31, [bHd, bW2], [bb])
                        tt("dve", xn[:, half * 512:(half + 1) * 512], pp[:], xt[:, half * 512:(half + 1) * 512], ALU.add,
                           [bb, bX], [bXN])
                    if not last:
                        P.dma("pool", xs[t * 128:(t + 1) * 128, :], xn[:], reads=[bXN], writes=[])
                    else:
                        sx, bS = statr.next()
                        act(junk[:], xn[:], AF.Square, [bXN], [bJ, bS], accum=sx[:, 0:1])
                        rstd_of(sx[:, 1:2], sx[:, 0:1], D, [bS], [bS])
                        stt("dve", xn[:], xn[:], sx[:, 1:2], gf[:], ALU.mult, ALU.mult, [bXN, bS, bGf], [bXN])
                        P.dma("pool", out[t * 128:(t + 1) * 128, :], xn[:], reads=[bXN])
            P.flush()

    phase_W()
    for l in range(DEPTH):
        phase_A(l)
        phase_B(l)
        phase_C(l)
        phase_D(l)
        phase_E1(l)
        phase_E2(l, l == DEPTH - 1)
    top.close()
    nc._n_emitted = P.ninst
    return nc


def host_inputs(inp, S, DEPTH):
    f = lambda a: np.ascontiguousarray(np.asarray(a, dtype=np.float32))
    w_in = f(inp["w_in"])[:DEPTH]
    w_in = np.concatenate([w_in, np.zeros((DEPTH, D, 1), np.float32)], axis=2)[:, :, _win_cols()]
    ropeA, ropeB = _rope_tables(S)
    shared = {
        "w_in": np.ascontiguousarray(w_in),
        "g_attn": _pk(f(inp["attn_norm"])[:DEPTH]),
        "w1k": f(inp["cmp_w1_k"])[:DEPTH], "w1v": f(inp["cmp_w1_v"])[:DEPTH],
        "w2k": f(inp["cmp_w2_k"])[:DEPTH], "w2v": f(inp["cmp_w2_v"])[:DEPTH],
        "posk": np.ascontiguousarray(f(inp["cmp_pos_k"])[:DEPTH].transpose(0, 2, 1)),
        "posv": np.ascontiguousarray(f(inp["cmp_pos_v"])[:DEPTH].transpose(0, 2, 1)),
        "wq": np.ascontiguousarray(f(inp["w_q_up"])[:DEPTH][:, :, _wq_cols()]),
        "g_q": _pk(f(inp["mla_q_norm"])[:DEPTH]),
        "wkv": np.ascontiguousarray(f(inp["w_kv_up"])[:DEPTH][:, :, _wkv_cols()]),
        "g_kv": _pk(f(inp["mla_kv_norm"])[:DEPTH]),
        "w_out": f(inp["w_out"])[:DEPTH],
        "g_mix": _pk(np.concatenate([f(inp["nsa_out_norm"])[:DEPTH], f(inp["mla_out_norm"])[:DEPTH]], axis=1)),
        "w_ff1": f(inp["w_ff1"])[:DEPTH],
        "g_mlp": _pk(f(inp["mlp_norm"])[:DEPTH]),
        "w_ff2": f(inp["w_ff2"])[:DEPTH],
        "g_fin": np.ascontiguousarray(np.tile(f(inp["final_norm"]).reshape(1, D), (128, 1))),
        "ropeA": ropeA, "ropeB": ropeB,
    }
    for k, v in _consts(S).items():
        shared["c_" + k] = v
    return shared


_CACHE = {}


def kernel(**inputs):
    x = np.asarray(inputs["x"], dtype=np.float32)
    B, S, _ = x.shape
    DEPTH = np.asarray(inputs["w_in"]).shape[0]
    key = (S, DEPTH)
    if key not in _CACHE:
        _CACHE[key] = build(S, DEPTH)
    nc = _CACHE[key]
    shared = host_inputs(inputs, S, DEPTH)
    in_maps = []
    for b in range(B):
        m = dict(shared)
        m["x"] = np.ascontiguousarray(x[b])
        in_maps.append(m)
    res = run_bass_kernel_spmd(nc, in_maps, core_ids=list(range(B)))
    return np.stack([np.asarray(r["out"], dtype=np.float32) for r in res.results], axis=0)
```
